# Optimizing a Trainium2 kernel written in Bass

```python
import math
import jax
import jax.numpy as jnp
from jax import lax
import numpy as np

D_MODEL = 1024
BATCH = 8
SEQ = 4096
DEPTH = 2

GRID_W = 64
CTX_LEN = 256

N_MIXERS = 4
GROUP_WIDTH = D_MODEL // N_MIXERS
GROUP_HEADS = 4

SGU_CHUNK = 128
SGU_GROUPS = GROUP_HEADS

DIFF_HEADS = GROUP_HEADS
DIFF_V_DIM = GROUP_WIDTH // DIFF_HEADS
DIFF_QK_DIM = DIFF_V_DIM // 2

HGRN_HEADS = GROUP_HEADS
HGRN_KEY_DIM = 64
HGRN_VAL_DIM = GROUP_WIDTH // HGRN_HEADS
SCAN_CHUNK = 32

MLA_HEADS = GROUP_HEADS
MLA_Q_LORA = 256
MLA_KV_LORA = 128
MLA_NOPE_DIM = 64
MLA_ROPE_DIM = 32
MLA_V_DIM = GROUP_WIDTH // MLA_HEADS

ROPE_BASE = 10000.0
Q_BLOCK = 128

N_EXPERTS = 32
TOP_K = 4
D_EXPERT = D_MODEL
SWIGLU_ALPHA = 1.702
SWIGLU_LIMIT = 7.0
MOE_BLOCK = 256

NORM_EPS = 1e-6
DEEPNORM_ALPHA = (2 * DEPTH) ** 0.25
DEEPNORM_BETA = (8 * DEPTH) ** -0.25

COL_SIZES = (
    2 * GROUP_WIDTH,
    DIFF_HEADS * 2 * DIFF_QK_DIM,
    DIFF_HEADS * 2 * DIFF_QK_DIM,
    DIFF_HEADS * DIFF_V_DIM,
    HGRN_HEADS * HGRN_KEY_DIM,
    HGRN_HEADS * HGRN_VAL_DIM,
    HGRN_HEADS * HGRN_KEY_DIM,
    HGRN_HEADS * HGRN_KEY_DIM,
    GROUP_WIDTH,
    MLA_Q_LORA,
    MLA_KV_LORA,
    MLA_ROPE_DIM,
)
IN_COLS = sum(COL_SIZES)

kernel_name = 'hybrid_parallel_group_dit_block'


def layer_norm(x, w=None, b=None):
    xf = x.astype(jnp.float32)
    mu = jnp.mean(xf, axis=-1, keepdims=True)
    xc = xf - mu
    y = xc * lax.rsqrt(jnp.mean(xc * xc, axis=-1, keepdims=True) + NORM_EPS)
    if w is not None:
        y = y * w + b
    return y.astype(x.dtype)


def rms_norm(x, w):
    xf = x.astype(jnp.float32)
    y = xf * lax.rsqrt(jnp.mean(xf * xf, axis=-1, keepdims=True) + NORM_EPS)
    return (y * w).astype(x.dtype)


def modulate(h, shift, scale):
    return h * (1.0 + scale) + shift


def to_heads(t, n_heads):
    bsz, n, _ = t.shape
    return t.reshape(bsz, n, n_heads, -1).transpose(0, 2, 1, 3)


def from_heads(t):
    bsz, nh, n, hd = t.shape
    return t.transpose(0, 2, 1, 3).reshape(bsz, n, nh * hd)


def split_cols(p):
    bounds = [int(b) for b in np.cumsum(COL_SIZES)[:-1]]
    return jnp.split(p, bounds, axis=-1)


def grid_angles(n, rot_dim):
    rows = n // GRID_W
    row = jnp.repeat(jnp.arange(rows, dtype=jnp.float32), GRID_W)
    col = jnp.tile(jnp.arange(GRID_W, dtype=jnp.float32), rows)
    axis_dim = rot_dim // 2
    inv_freq = ROPE_BASE ** (-jnp.arange(0, axis_dim, 2, dtype=jnp.float32) / axis_dim)
    return row[:, None] * inv_freq, col[:, None] * inv_freq


def rope_1d(x, ang):
    cos = jnp.cos(ang).astype(x.dtype)
    sin = jnp.sin(ang).astype(x.dtype)
    x1, x2 = jnp.split(x, 2, axis=-1)
    return jnp.concatenate([x1 * cos - x2 * sin, x2 * cos + x1 * sin], axis=-1)


def axial_rope(x, ang_row, ang_col):
    xr, xc = jnp.split(x, 2, axis=-1)
    return jnp.concatenate([rope_1d(xr, ang_row), rope_1d(xc, ang_col)], axis=-1)


def sweep_query_blocks(attend, q):
    *lead, n, hd = q.shape
    nb = n // Q_BLOCK
    qb = jnp.moveaxis(q.reshape(*lead, nb, Q_BLOCK, hd), -3, 0)
    out = jnp.moveaxis(lax.map(attend, qb), 0, -3)
    return out.reshape(*out.shape[:-3], n, out.shape[-1])


def softmax_attend(q, k, v, scale):
    s = jnp.einsum('bhqd,bhsd->bhqs', q, k).astype(jnp.float32) * scale
    p = jax.nn.softmax(s, axis=-1)
    return jnp.einsum('bhqs,bhsv->bhqv', p.astype(v.dtype), v)


def diff_attend(q, k, v, lam, scale):
    s = jnp.einsum('bhmqd,bhmsd->bhmqs', q, k).astype(jnp.float32) * scale
    p = jax.nn.softmax(s, axis=-1)
    a = p[:, :, 0] - lam * p[:, :, 1]
    return jnp.einsum('bhqs,bhsv->bhqv', a.astype(v.dtype), v)


def diff_qk_heads(t):
    bsz, n, _ = t.shape
    return t.reshape(bsz, n, DIFF_HEADS, 2, DIFF_QK_DIM).transpose(0, 2, 3, 1, 4)


def spatial_gating(uv, ln_w, ln_b, w_s, b_s):
    uv = jax.nn.gelu(uv, approximate=False)
    u, v = jnp.split(uv, 2, axis=-1)
    v = layer_norm(v, ln_w, ln_b)
    bsz, n, _ = v.shape
    v = v.reshape(bsz, n // SGU_CHUNK, SGU_CHUNK, SGU_GROUPS, -1)
    v = jnp.einsum('gts,bnsgc->bntgc', w_s, v) + b_s.T[None, None, :, :, None]
    return u * v.reshape(bsz, n, -1)


def differential_attention(q, k, v, qc, kc, vc, lam_p, subln_w, lam_init, ang, need_ctx):
    scale = DIFF_QK_DIM ** -0.5
    lam_p = lam_p.astype(jnp.float32)
    lam = jnp.exp(jnp.sum(lam_p[0] * lam_p[1])) - jnp.exp(jnp.sum(lam_p[2] * lam_p[3])) + lam_init

    def post(o):
        return from_heads(rms_norm(o, subln_w) * (1.0 - lam_init))

    kc_h = diff_qk_heads(kc)
    vc_h = to_heads(vc, DIFF_HEADS)
    k_all = jnp.concatenate([kc_h, axial_rope(diff_qk_heads(k), *ang)], axis=3)
    v_all = jnp.concatenate([vc_h, to_heads(v, DIFF_HEADS)], axis=2)
    q_h = axial_rope(diff_qk_heads(q), *ang)
    o = sweep_query_blocks(lambda qb: diff_attend(qb, k_all, v_all, lam, scale), q_h)
    o_ctx = post(diff_attend(diff_qk_heads(qc), kc_h, vc_h, lam, scale)) if need_ctx else None
    return post(o), o_ctx


def forget_gate(f_raw, lb_vec):
    lb_h = lb_vec.reshape(HGRN_HEADS, 1, HGRN_KEY_DIM)
    f = lb_h + (1.0 - lb_h) * jax.nn.sigmoid(f_raw.astype(jnp.float32))
    return 1.0 - f, jnp.log(f)


def gla_chunked(q, k, v, log_f, s0):
    bsz, nh, n, _ = q.shape
    dv = v.shape[-1]
    nc = n // SCAN_CHUNK

    def chunk(t):
        return t.reshape(bsz, nh, nc, SCAN_CHUNK, t.shape[-1])

    q, k, v, log_f = chunk(q), chunk(k), chunk(v), chunk(log_f)
    b = jnp.cumsum(log_f, axis=3)
    b_last = b[:, :, :, -1:, :]
    q_dec = q * jnp.exp(b)
    scores = jnp.einsum('bhnik,bhnjk->bhnij', q_dec, k * jnp.exp(-b))
    lower = jnp.tril(jnp.ones((SCAN_CHUNK, SCAN_CHUNK), dtype=bool))
    o_intra = jnp.einsum('bhnij,bhnjv->bhniv', jnp.where(lower, scores, 0.0), v)
    d_state = jnp.einsum('bhnjk,bhnjv->bhnkv', k * jnp.exp(b_last - b), v)
    chunk_decay = jnp.exp(b_last[:, :, :, 0, :])

    def step(state, inp):
        dec, ds = inp
        return dec[..., None] * state + ds, state

    s_final, s_enter = lax.scan(step, s0, (jnp.moveaxis(chunk_decay, 2, 0), jnp.moveaxis(d_state, 2, 0)))
    o_inter = jnp.einsum('bhnik,bhnkv->bhniv', q_dec, jnp.moveaxis(s_enter, 0, 2))
    return (o_intra + o_inter).reshape(bsz, nh, n, dv), s_final


def bidirectional_scan(q, v, k_f, log_f_f, k_b, log_f_b, s0_f, s0_b):
    def flip(t):
        return jnp.flip(t, axis=2)

    o_f, s_f = gla_chunked(q, k_f, v, log_f_f, s0_f)
    o_b, s_b = gla_chunked(flip(q), flip(k_b), flip(v), flip(log_f_b), s0_b)
    return o_f + flip(o_b), s_f, s_b


def hgrn2_bidirectional(q, i, ff, fb, g, qc, ic, ffc, fbc, gc, lb, norm_w, need_ctx):
    def prep(q_, i_, ff_, fb_):
        qh = jax.nn.silu(to_heads(q_, HGRN_HEADS).astype(jnp.float32))
        vh = to_heads(i_, HGRN_HEADS).astype(jnp.float32)
        k_f, log_f_f = forget_gate(to_heads(ff_, HGRN_HEADS), lb[0])
        k_b, log_f_b = forget_gate(to_heads(fb_, HGRN_HEADS), lb[1])
        return qh, vh, k_f, log_f_f, k_b, log_f_b

    def readout(o, g_):
        return (from_heads(rms_norm(o, norm_w)) * jax.nn.silu(g_.astype(jnp.float32))).astype(g_.dtype)

    s0 = jnp.zeros((q.shape[0], HGRN_HEADS, HGRN_KEY_DIM, HGRN_VAL_DIM), jnp.float32)
    o_c, s_f, s_b = bidirectional_scan(*prep(qc, ic, ffc, fbc), s0, s0)
    o, _, _ = bidirectional_scan(*prep(q, i, ff, fb), s_f, s_b)
    return readout(o, g), (readout(o_c, gc) if need_ctx else None)


def latent_attention(cq, ckv, kr, cqc, ckvc, krc, q_norm_w, w_uq, kv_norm_w, w_ukv, ang, need_ctx):
    scale = (MLA_NOPE_DIM + MLA_ROPE_DIM) ** -0.5

    def queries(cq_, rotate):
        qf = to_heads(rms_norm(cq_, q_norm_w) @ w_uq, MLA_HEADS)
        q_nope, q_rope = qf[..., :MLA_NOPE_DIM], qf[..., MLA_NOPE_DIM:]
        if rotate:
            q_rope = axial_rope(q_rope, *ang)
        return jnp.concatenate([q_nope, q_rope], axis=-1)

    def keys_values(ckv_, kr_, rotate):
        kvf = to_heads(rms_norm(ckv_, kv_norm_w) @ w_ukv, MLA_HEADS)
        k_nope, v = kvf[..., :MLA_NOPE_DIM], kvf[..., MLA_NOPE_DIM:]
        k_rope = kr_[:, None]
        if rotate:
            k_rope = axial_rope(k_rope, *ang)
        k_rope = jnp.broadcast_to(k_rope, k_nope.shape[:-1] + (MLA_ROPE_DIM,))
        return jnp.concatenate([k_nope, k_rope], axis=-1), v

    kc, vc = keys_values(ckvc, krc, False)
    k, v = keys_values(ckv, kr, True)
    k_all = jnp.concatenate([kc, k], axis=2)
    v_all = jnp.concatenate([vc, v], axis=2)
    o = sweep_query_blocks(lambda qb: softmax_attend(qb, k_all, v_all, scale), queries(cq, True))
    o_ctx = from_heads(softmax_attend(queries(cqc, False), kc, vc, scale)) if need_ctx else None
    return from_heads(o), o_ctx


def hybrid_mixer(h, hc, w_in, w_out, sgu_ln_w, sgu_ln_b, sgu_w, sgu_b,
                 diff_lam, diff_subln_w, lam_init, hgrn_lb, hgrn_norm_w,
                 mla_q_norm_w, mla_w_uq, mla_kv_norm_w, mla_w_ukv,
                 ang_diff, ang_mla, need_ctx):
    (a_uv, b_q, b_k, b_v, c_q, c_i, c_ff, c_fb, c_g, d_cq, d_ckv, d_kr) = split_cols(h @ w_in)
    (a_uvc, b_qc, b_kc, b_vc, c_qc, c_ic, c_ffc, c_fbc, c_gc, d_cqc, d_ckvc, d_krc) = split_cols(hc @ w_in)

    o_a = spatial_gating(a_uv, sgu_ln_w, sgu_ln_b, sgu_w, sgu_b)
    o_b, o_bc = differential_attention(b_q, b_k, b_v, b_qc, b_kc, b_vc, diff_lam, diff_subln_w,
                                       lam_init, ang_diff, need_ctx)
    o_c, o_cc = hgrn2_bidirectional(c_q, c_i, c_ff, c_fb, c_g, c_qc, c_ic, c_ffc, c_fbc, c_gc,
                                    hgrn_lb, hgrn_norm_w, need_ctx)
    o_d, o_dc = latent_attention(d_cq, d_ckv, d_kr, d_cqc, d_ckvc, d_krc, mla_q_norm_w, mla_w_uq,
                                 mla_kv_norm_w, mla_w_ukv, ang_mla, need_ctx)
    dt = h.dtype
    y = jnp.concatenate([o_a.astype(dt), o_b.astype(dt), o_c.astype(dt), o_d.astype(dt)], axis=-1) @ w_out
    if not need_ctx:
        return y, None
    o_ac = spatial_gating(a_uvc, sgu_ln_w, sgu_ln_b, sgu_w, sgu_b)
    yc = jnp.concatenate([o_ac.astype(dt), o_bc.astype(dt), o_cc.astype(dt), o_dc.astype(dt)], axis=-1) @ w_out
    return y, yc


def clamped_swiglu(hid):
    glu, lin = hid[..., ::2], hid[..., 1::2]
    glu = jnp.minimum(glu, SWIGLU_LIMIT)
    lin = jnp.clip(lin, -SWIGLU_LIMIT, SWIGLU_LIMIT)
    return glu * jax.nn.sigmoid(SWIGLU_ALPHA * glu) * (lin + 1.0)


def moe_ffn(h, router_w, router_b, w1, b1, w2, b2):
    n_tok, d = h.shape
    logits = (h @ router_w + router_b).astype(jnp.float32)
    top_val, top_idx = lax.top_k(logits, TOP_K)
    gate = jax.nn.softmax(top_val, axis=-1)
    n_assign = n_tok * TOP_K
    flat_e = top_idx.reshape(-1)
    order = jnp.argsort(flat_e)
    sorted_e = flat_e[order]
    sorted_tok = (order // TOP_K).astype(jnp.int32)
    sorted_gate = gate.reshape(-1)[order]
    counts = jnp.bincount(flat_e, length=N_EXPERTS)
    padded = (counts + MOE_BLOCK - 1) // MOE_BLOCK * MOE_BLOCK
    pad_end = jnp.cumsum(padded)
    pad_start = pad_end - padded
    grp_start = jnp.cumsum(counts) - counts
    dest = pad_start[sorted_e] + jnp.arange(n_assign) - grp_start[sorted_e]
    n_blocks = -(-n_assign // MOE_BLOCK) + N_EXPERTS
    n_rows = n_blocks * MOE_BLOCK
    buf_tok = jnp.zeros((n_rows,), jnp.int32).at[dest].set(sorted_tok)
    buf_gate = jnp.zeros((n_rows,), jnp.float32).at[dest].set(sorted_gate)
    block_e = jnp.minimum(jnp.searchsorted(pad_end, jnp.arange(n_blocks) * MOE_BLOCK, side='right'),
                          N_EXPERTS - 1)

    def expert_block(args):
        tok_b, gate_b, e = args
        xb = h[tok_b]
        a = clamped_swiglu(xb @ w1[e] + b1[e])
        return (a @ w2[e] + b2[e]) * gate_b[:, None].astype(h.dtype)

    ys = lax.map(expert_block, (buf_tok.reshape(n_blocks, MOE_BLOCK),
                                buf_gate.reshape(n_blocks, MOE_BLOCK), block_e))
    return jnp.zeros_like(h).at[buf_tok].add(ys.reshape(n_rows, d).astype(h.dtype))


def setup_inputs(seed: int = 0) -> dict:
    key = jax.random.key(seed)
    ks = jax.random.split(key, 30)
    L, D, E, F = DEPTH, D_MODEL, N_EXPERTS, D_EXPERT

    def nrm(k, shape, s):
        return jax.random.normal(k, shape, jnp.float32) * s

    def gain(k, shape):
        return 1.0 + nrm(k, shape, 0.02)

    return {
        'x': nrm(ks[0], (BATCH, SEQ, D), 1.0),
        'c': nrm(ks[1], (BATCH, D), 1.0),
        'ctx': nrm(ks[2], (BATCH, CTX_LEN, D), 1.0),
        'c_ctx': nrm(ks[3], (D,), 1.0),
        'ada_w': nrm(ks[4], (L, D, 6 * D), 0.5 * D ** -0.5),
        'ada_b': nrm(ks[5], (L, 6 * D), 0.02),
        'w_in': nrm(ks[6], (L, D, IN_COLS), D ** -0.5),
        'w_out': nrm(ks[7], (L, D, D), DEEPNORM_BETA * D ** -0.5),
        'sgu_ln_w': gain(ks[8], (L, GROUP_WIDTH)),
        'sgu_ln_b': nrm(ks[9], (L, GROUP_WIDTH), 0.02),
        'sgu_w': nrm(ks[10], (L, SGU_GROUPS, SGU_CHUNK, SGU_CHUNK), SGU_CHUNK ** -0.5),
        'sgu_b': gain(ks[11], (L, SGU_GROUPS, SGU_CHUNK)),
        'diff_lambda': nrm(ks[12], (L, 4, DIFF_QK_DIM), 0.1),
        'diff_subln_w': gain(ks[13], (L, DIFF_V_DIM)),
        'hgrn_lower_bounds': nrm(ks[14], (L, 2, HGRN_HEADS * HGRN_KEY_DIM), 0.1),
        'hgrn_norm_w': gain(ks[15], (L, HGRN_VAL_DIM)),
        'mla_q_norm_w': gain(ks[16], (L, MLA_Q_LORA)),
        'mla_w_uq': nrm(ks[17], (L, MLA_Q_LORA, MLA_HEADS * (MLA_NOPE_DIM + MLA_ROPE_DIM)), MLA_Q_LORA ** -0.5),
        'mla_kv_norm_w': gain(ks[18], (L, MLA_KV_LORA)),
        'mla_w_ukv': nrm(ks[19], (L, MLA_KV_LORA, MLA_HEADS * (MLA_NOPE_DIM + MLA_V_DIM)), MLA_KV_LORA ** -0.5),
        'ln_mix_w': gain(ks[20], (L, D)),
        'ln_mix_b': nrm(ks[21], (L, D), 0.02),
        'ln_ffn_w': gain(ks[22], (L, D)),
        'ln_ffn_b': nrm(ks[23], (L, D), 0.02),
        'router_w': nrm(ks[24], (L, D, E), D ** -0.5),
        'router_b': nrm(ks[25], (L, E), 0.01),
        'expert_w1': nrm(ks[26], (L, E, D, 2 * F), D ** -0.5),
        'expert_b1': nrm(ks[27], (L, E, 2 * F), 0.02),
        'expert_w2': nrm(ks[28], (L, E, F, D), DEEPNORM_BETA * F ** -0.5),
        'expert_b2': nrm(ks[29], (L, E, D), 0.02),
    }


def reference(x, c, ctx, c_ctx, ada_w, ada_b, w_in, w_out, sgu_ln_w, sgu_ln_b, sgu_w, sgu_b,
              diff_lambda, diff_subln_w, hgrn_lower_bounds, hgrn_norm_w,
              mla_q_norm_w, mla_w_uq, mla_kv_norm_w, mla_w_ukv,
              ln_mix_w, ln_mix_b, ln_ffn_w, ln_ffn_b,
              router_w, router_b, expert_w1, expert_b1, expert_w2, expert_b2):
    bsz, n_lat, d = x.shape
    n_ctx = ctx.shape[1]
    ang_diff = grid_angles(n_lat, DIFF_QK_DIM)
    ang_mla = grid_angles(n_lat, MLA_ROPE_DIM)
    lb_all = jax.nn.softmax(hgrn_lower_bounds.astype(jnp.float32), axis=0)
    lb_all = jnp.cumsum(lb_all, axis=0) - lb_all[0]
    silu_c = jax.nn.silu(c)
    silu_cc = jax.nn.silu(c_ctx)
    xc = ctx
    for l in range(DEPTH):
        need_ctx = l < DEPTH - 1
        lam_init = 0.8 - 0.6 * math.exp(-0.3 * l)
        mod = (silu_c @ ada_w[l] + ada_b[l])[:, None, :]
        mod_c = silu_cc @ ada_w[l] + ada_b[l]
        sh1, sc1, g1, sh2, sc2, g2 = jnp.split(mod, 6, axis=-1)
        csh1, csc1, cg1, csh2, csc2, cg2 = jnp.split(mod_c, 6, axis=-1)

        h = modulate(layer_norm(x), sh1, sc1)
        hc = modulate(layer_norm(xc), csh1, csc1)
        y, yc = hybrid_mixer(h, hc, w_in[l], w_out[l], sgu_ln_w[l], sgu_ln_b[l], sgu_w[l], sgu_b[l],
                             diff_lambda[l], diff_subln_w[l], lam_init, lb_all[l], hgrn_norm_w[l],
                             mla_q_norm_w[l], mla_w_uq[l], mla_kv_norm_w[l], mla_w_ukv[l],
                             ang_diff, ang_mla, need_ctx)
        x = layer_norm(DEEPNORM_ALPHA * x + g1 * y, ln_mix_w[l], ln_mix_b[l])
        h2 = modulate(layer_norm(x), sh2, sc2)
        if need_ctx:
            xc = layer_norm(DEEPNORM_ALPHA * xc + cg1 * yc, ln_mix_w[l], ln_mix_b[l])
            h2c = modulate(layer_norm(xc), csh2, csc2)
            tokens = jnp.concatenate([h2.reshape(-1, d), h2c.reshape(-1, d)], axis=0)
        else:
            tokens = h2.reshape(-1, d)
        f_out = moe_ffn(tokens, router_w[l], router_b[l], expert_w1[l], expert_b1[l],
                        expert_w2[l], expert_b2[l])
        y2 = f_out[:bsz * n_lat].reshape(bsz, n_lat, d)
        x = layer_norm(DEEPNORM_ALPHA * x + g2 * y2, ln_ffn_w[l], ln_ffn_b[l])
        if need_ctx:
            y2c = f_out[bsz * n_lat:].reshape(bsz, n_ctx, d)
            xc = layer_norm(DEEPNORM_ALPHA * xc + cg2 * y2c, ln_ffn_w[l], ln_ffn_b[l])
    return x
```

```python
import math
from contextlib import ExitStack
from concourse.bass_utils import run_bass_kernel_spmd
import numpy as np
import concourse.bass as bass
import concourse.mybir as mybir

F32 = mybir.dt.float32
BF16 = mybir.dt.bfloat16
I32 = mybir.dt.int32
U32 = mybir.dt.uint32
AF = mybir.ActivationFunctionType
ALU = mybir.AluOpType
AX = mybir.AxisListType

_DT_SIZE = {}


def dt_size(dt):
    s = _DT_SIZE.get(dt)
    if s is None:
        n = str(dt)
        if '64' in n:
            s = 8
        elif '32' in n:
            s = 4
        elif '16' in n:
            s = 2
        else:
            s = 1
        _DT_SIZE[dt] = s
    return s


EPOCH = 30000


class Sched:
    ENG = ['pe', 'dve', 'act', 'pool', 'sp']

    def __init__(self, nc, lanes=None):
        self.nc = nc
        self.ops = {e: [] for e in self.ENG}
        self.cnt = {}
        self.seen = {e: {} for e in self.ENG}
        self.wr = {}
        self.rd = {}
        lanes = lanes or {'sp': 8, 'pool': 6, 'act': 2}
        self.lanes = {qn: [f'd_{qn}{i}' for i in range(n)] for qn, n in lanes.items()}
        self.lane_rr = {qn: 0 for qn in lanes}
        self.signal = {}
        self.psum_names = set()

    def box(self, ap):
        t = ap.tensor
        name = t.name
        pat = ap.ap
        off = ap.offset
        sp = type(t).__name__
        if sp.startswith('DRam') or sp.startswith('Dram'):
            ext = 1
            for st, c in pat:
                ext += abs(st) * (c - 1)
            return name, (0, 1, off, off + ext)
        if sp.startswith('PSum'):
            self.psum_names.add(name)
            return name, (0, 128, 0, 1 << 30)
        shp = list(t.shape)
        F = 1
        for d in shp[1:]:
            F *= d
        ts_ = dt_size(t.dtype)
        vs = dt_size(ap.dtype)
        Fv = F * ts_ // vs if vs != ts_ else F
        pst, pc = pat[0]
        if pc > 1:
            assert pst == Fv, (name, pat, Fv)
        p0 = off // Fv
        f0 = off % Fv
        ext = 1
        for st, c in pat[1:]:
            ext += abs(st) * (c - 1)
        return name, (p0, p0 + pc, f0 * vs, (f0 + ext) * vs)

    @staticmethod
    def _ov(a, b):
        return a[0] < b[1] and b[0] < a[1] and a[2] < b[3] and b[2] < a[3]

    @staticmethod
    def _cov(a, b):
        return a[0] <= b[0] and a[1] >= b[1] and a[2] <= b[2] and a[3] >= b[3]

    def _deps(self, reads, writes):
        toks = {}

        def add(k, v):
            if toks.get(k, -1) < v:
                toks[k] = v
        for nm, bx in reads:
            for wb, (k, v) in self.wr.get(nm, ()):
                if self._ov(bx, wb):
                    add(k, v)
        for nm, bx in writes:
            for wb, (k, v) in self.wr.get(nm, ()):
                if self._ov(bx, wb):
                    add(k, v)
            for (rb, k), v in self.rd.get(nm, {}).items():
                if self._ov(bx, rb):
                    add(k, v)
        return toks

    def _record(self, reads, writes, tok):
        k, v = tok
        for nm, bx in reads:
            self.rd.setdefault(nm, {})[(bx, k)] = v
        for nm, bx in writes:
            lst = [(wb, t) for wb, t in self.wr.get(nm, ()) if not self._cov(bx, wb)]
            lst.append((bx, tok))
            self.wr[nm] = lst
            d = self.rd.get(nm)
            if d:
                for key in [key for key in d if self._cov(bx, key[0])]:
                    del d[key]

    def _waits_for(self, eng, toks, skip_key=None):
        out = []
        seen = self.seen[eng]
        for k, v in toks.items():
            if k == skip_key:
                continue
            if seen.get(k, -1) >= v:
                continue
            seen[k] = v
            out.append((k, v))
            self.signal.setdefault(k, set()).add(v)
        return out

    def _boxes(self, reads, writes):
        rb = []
        wb = []
        for a in reads:
            b = self.box(a)
            (wb if b[0] in self.psum_names else rb).append(b)
        for a in writes:
            wb.append(self.box(a))
        return rb, wb

    def op(self, eng, fn, reads=(), writes=()):
        rb, wb = self._boxes(reads, writes)
        toks = self._deps(rb, wb)
        idx = self.cnt.get(eng, 0)
        self.cnt[eng] = idx + 1
        waits = self._waits_for(eng, toks, skip_key='pe' if eng == 'pe' else None)
        self.ops[eng].append(['op', fn, waits, eng, idx])
        self._record(rb, wb, (eng, idx))

    def dma(self, qn, out, in_, extra_reads=(), **kw):
        rb, wb = self._boxes([in_] + list(extra_reads), [out])
        toks = self._deps(rb, wb)
        lanes = self.lanes[qn]
        li = self.lane_rr[qn]
        self.lane_rr[qn] = (li + 1) % len(lanes)
        lane = lanes[li]
        idx = self.cnt.get(lane, 0)
        self.cnt[lane] = idx + 1
        if idx > 0 and toks.get(lane, -1) < idx - 1:
            toks[lane] = idx - 1
        waits = self._waits_for(qn, toks)
        self.ops[qn].append(['dma', (out, in_, kw), waits, lane, idx])
        self._record(rb, wb, (lane, idx))

    def barrier(self, final=False):
        bg = (lambda kk: False) if final else (lambda kk: kk.startswith('d_pool'))
        toks = {k: c - 1 for k, c in self.cnt.items() if c > 0 and not bg(k)}
        for eng in self.ENG:
            waits = self._waits_for(eng, dict(toks))
            self.ops[eng].append(['wait', None, waits, None, None])
        keep = {}
        for nm, lst in self.wr.items():
            l2 = [(b, tk) for b, tk in lst if bg(tk[0])]
            if l2:
                keep[nm] = l2
        self.wr = keep
        self.rd = {}

    def emit(self):
        self.barrier(final=True)
        nc = self.nc
        rank = {}
        sems = {}
        for k, idxs in self.signal.items():
            isdma = k.startswith('d_')
            if isdma:
                n = self.cnt[k]
                rank[k] = None
                per = EPOCH // 16
                for ep in range((n + per - 1) // per):
                    sems[(k, ep)] = nc.alloc_semaphore(f'sm_{k}_{ep}')
            else:
                s = sorted(idxs)
                rank[k] = {ix: r for r, ix in enumerate(s)}
                for ep in range((len(s) + EPOCH - 1) // EPOCH):
                    sems[(k, ep)] = nc.alloc_semaphore(f'sm_{k}_{ep}')
        for k, n in self.cnt.items():
            if k.startswith('d_') and k not in rank:
                rank[k] = None
                per = EPOCH // 16
                for ep in range((n + per - 1) // per):
                    sems[(k, ep)] = nc.alloc_semaphore(f'sm_{k}_{ep}')
        self.n_sems = len(sems)

        def resolve(k, v):
            if rank[k] is None:
                per = EPOCH // 16
                return sems[(k, v // per)], ((v % per) + 1) * 16
            r = rank[k][v]
            return sems[(k, r // EPOCH)], (r % EPOCH) + 1

        def run_list(e, lst):
            for kind, payload, waits, mk, mi in lst:
                for k, v in waits:
                    s, val = resolve(k, v)
                    e.wait_ge(s, val)
                if kind == 'op':
                    ins = payload(e)
                    rk = rank.get(mk)
                    if rk is not None and mi in rk:
                        s, val = resolve(mk, mi)
                        ins.then_inc(s, 1)
                elif kind == 'dma':
                    out, in_, kw = payload
                    s, val = resolve(mk, mi)
                    e.dma_start(out=out, in_=in_, **kw).then_inc(s, 16)

        ops = self.ops
        with nc.Block() as block:
            @block.tensor
            def _(e):
                run_list(e, ops['pe'])

            @block.vector
            def _(e):
                run_list(e, ops['dve'])

            @block.scalar
            def _(e):
                run_list(e, ops['act'])

            @block.gpsimd
            def _(e):
                run_list(e, ops['pool'])

            @block.sync
            def _(e):
                run_list(e, ops['sp'])

    def mm(self, out, lhsT, rhs, start=True, stop=True):
        rd = [lhsT, rhs]
        self.op('pe', lambda e: e.matmul(out, lhsT, rhs, start=start, stop=stop), rd, [out])

    def tr(self, out, in_, ident):
        self.op('pe', lambda e: e.transpose(out, in_, ident), [in_, ident], [out])

    def act(self, out, in_, func, bias=None, scale=None, accum_out=None):
        kw = {}
        rd = [in_]
        wr = [out]
        if bias is not None:
            kw['bias'] = bias
            if not isinstance(bias, (int, float)):
                rd.append(bias)
        if scale is not None:
            kw['scale'] = scale
            if not isinstance(scale, (int, float)):
                rd.append(scale)
        if accum_out is not None:
            kw['accum_out'] = accum_out
            wr.append(accum_out)
        self.op('act', lambda e: e.activation(out, in_, func, **kw), rd, wr)

    def copy(self, eng, out, in_):
        if eng == 'act':
            self.op('act', lambda e: e.copy(out, in_), [in_], [out])
        else:
            self.op(eng, lambda e: e.tensor_copy(out, in_), [in_], [out])

    def tt(self, eng, out, in0, in1, op):
        self.op(eng, lambda e: e.tensor_tensor(out, in0, in1, op), [in0, in1], [out])

    def ts(self, eng, out, in0, s1, op0, s2=None, op1=None, accum_out=None):
        rd = [in0]
        if not isinstance(s1, (int, float)):
            rd.append(s1)
        if s2 is not None and not isinstance(s2, (int, float)):
            rd.append(s2)
        wr = [out]
        kw = {}
        if op1 is not None:
            kw['op1'] = op1
        if accum_out is not None:
            kw['accum_out'] = accum_out
            wr.append(accum_out)
        self.op(eng, lambda e: e.tensor_scalar(out, in0, s1, s2, op0, **kw), rd, wr)

    def stt(self, eng, out, in0, scalar, in1, op0, op1):
        rd = [in0, in1]
        if not isinstance(scalar, (int, float)):
            rd.append(scalar)
        self.op(eng, lambda e: e.scalar_tensor_tensor(out, in0, scalar, in1, op0, op1), rd, [out])

    def memset(self, eng, out, val):
        self.op(eng, lambda e: e.memset(out, val), [], [out])

D = 1024
T = 4096
CT = 256
NTOK = T + CT
NT = NTOK // 128
DEPTH = 2
NEXP = 32
ALPHA = (2 * DEPTH) ** 0.25
EPS = 1e-6
CA = (0, 512)
CBQ, CBK, CBV = 512, 768, 1024
CC0 = 1280
CD0 = 2560
SW_ALPHA = 1.702
SW_LIM = 7.0


def host_consts():
    c = {}
    c['ident'] = np.eye(128, dtype=np.float32)
    pos = np.arange(T)
    row = (pos // 64).astype(np.float32)
    col = (pos % 64).astype(np.float32)
    inv = (10000.0 ** (-np.arange(0, 16, 2, dtype=np.float32) / 16.0)).astype(np.float32)
    ar = row[:, None] * inv[None, :]
    ac = col[:, None] * inv[None, :]
    ang = np.concatenate([ar, ac], axis=1).astype(np.float32)
    c['rope'] = np.stack([np.cos(ang), np.sin(ang)], axis=1).astype(np.float32)
    s = np.arange(128)[:, None]
    t = np.arange(128)[None, :]
    same = (s // 32) == (t // 32)
    masks = np.stack([same & (s <= t), same & (s >= t), same & (s > t), same & (s < t)], 0)
    c['gmask'] = masks.astype(np.float32)
    ci = (np.arange(128)[:, None] // 32 == np.arange(4)[None, :]).astype(np.float32)
    c['cind'] = ci
    sel = np.zeros((2, 2, 128), np.float32)
    sel[0, 0, :] = 1.0
    sel[1, 1, :] = 1.0
    c['sel'] = sel
    return c


class K:
    pass


_UID = [0]


def UN(name):
    _UID[0] += 1
    return f"{name}_u{_UID[0]}"


def build_program(cfg):
    layers = cfg.get('layers', [0, 1])
    nexp = cfg.get('nexp', NEXP)
    phases = cfg.get('phases', 'ALL')
    dbg = cfg.get('dbg', [])
    nc = bass.Bass("TRN2", target_bir_lowering=False)
    S = Sched(nc)
    k = K()
    k.nc, k.S, k.cfg, k.nexp = nc, S, cfg, nexp
    k.last_layer = layers[-1]

    def din(name, shape, dt=F32):
        return nc.dram_tensor(name, list(shape), dt, kind="ExternalInput").ap()

    def dscr(name, shape, dt):
        return nc.dram_tensor(name, list(shape), dt, kind="Internal").ap()

    k.x = din('x', [T, D])
    k.ctx = din('ctx', [CT, D])
    k.cvec = din('cvec', [2, D])
    k.ada_w = din('ada_w', [DEPTH, D, 6 * D])
    k.ada_b = din('ada_b', [DEPTH, 6 * D])
    k.w_in = din('w_in', [DEPTH, D, 2976])
    k.w_out = din('w_out', [DEPTH, D, D])
    k.sgu_ln_w = din('sgu_ln_w', [DEPTH, 256])
    k.sgu_ln_b = din('sgu_ln_b', [DEPTH, 256])
    k.sgu_w = din('sgu_w', [DEPTH, 4, 128, 128])
    k.sgu_b = din('sgu_b', [DEPTH, 4, 128])
    k.diff_lambda = din('diff_lambda', [DEPTH, 4, 32])
    k.diff_subln_w = din('diff_subln_w', [DEPTH, 64])
    k.hlb = din('hgrn_lower_bounds', [DEPTH, 2, 256])
    k.hgrn_norm_w = din('hgrn_norm_w', [DEPTH, 64])
    k.mla_q_norm_w = din('mla_q_norm_w', [DEPTH, 256])
    k.mla_w_uq = din('mla_w_uq', [DEPTH, 256, 384])
    k.mla_kv_norm_w = din('mla_kv_norm_w', [DEPTH, 128])
    k.mla_w_ukv = din('mla_w_ukv', [DEPTH, 128, 512])
    k.ln_mix_w = din('ln_mix_w', [DEPTH, D])
    k.ln_mix_b = din('ln_mix_b', [DEPTH, D])
    k.ln_ffn_w = din('ln_ffn_w', [DEPTH, D])
    k.ln_ffn_b = din('ln_ffn_b', [DEPTH, D])
    k.router_w = din('router_w', [DEPTH, D, NEXP])
    k.router_b = din('router_b', [DEPTH, NEXP])
    NL = len(layers)
    k.w1 = din('expert_w1', [NL, nexp, D, 2 * D])
    k.b1 = din('expert_b1', [NL, nexp, 2 * D])
    k.w2 = din('expert_w2', [NL, nexp, D, D])
    k.b2 = din('expert_b2', [NL, nexp, D])
    k.c_ident = din('ident', [128, 128])
    k.c_rope = din('rope', [T, 2, 16])
    k.c_gmask = din('gmask', [4, 128, 128])
    k.c_cind = din('cind', [128, 4])
    k.c_sel = din('sel', [2, 2, 128])
    k.y = nc.dram_tensor('y', [T, D], F32, kind="ExternalOutput").ap()

    k.hT_d = dscr('hT_d', [NT, 128, 8, 128], BF16)
    k.cat_d = dscr('cat_d', [NTOK, D], BF16)
    k.xs_d = dscr('xs_d', [NTOK, D], F32)
    k.oF_d = dscr('oF_d', [NT, 64, 4, 128], F32)
    k.w1b_d = dscr('w1b_d', [NL, nexp, D, 2 * D], BF16)
    k.w2b_d = dscr('w2b_d', [NL, nexp, D, D], BF16)
    k.dbg_outs = {}

    def dbg_copy(name, src):
        if name not in dbg:
            return
        o = nc.dram_tensor('dbg_' + name, list(src.shape), src.dtype, kind="ExternalOutput").ap()
        k.dbg_outs[name] = o
        S.dma('sp', o, src)
    k.dbg_copy = dbg_copy

    with ExitStack() as es0:
        def sbp(name, shape, dt):
            return es0.enter_context(nc.sbuf_tensor(UN(name), list(shape), dt)).ap()
        k.ident_f = sbp('ident_f', [128, 128], F32)
        k.ident_b = sbp('ident_b', [128, 128], BF16)
        k.nhalf = sbp('nhalf', [128, 1], F32)
        S.dma('sp', k.ident_f, k.c_ident)
        S.copy('dve', k.ident_b, k.ident_f)
        k.epsb = sbp('epsb', [128, 1], F32)
        S.memset('dve', k.epsb, EPS)
        k.modT_l = {l: sbp(f'modT{l}', [128, 48, 2], F32) for l in layers}
        k.gates = sbp('gates', [128, NT, NEXP], F32)
        if phases == 'ALL' or '0' in phases:
            for l in layers:
                k.l = l
                k.modT = k.modT_l[l]
                phase_mod(k)
        k.conv_list = [(li_, e) for li_ in range(NL) for e in range(nexp)] if (phases == 'ALL' or 'M' in phases) else []

        def conv_step(gate=None, n=1):
            for _ in range(n):
                if not k.conv_list:
                    return
                li_, e = k.conv_list.pop(0)
                er = [gate] if gate is not None else []
                for h in range(2):
                    S.dma('pool', k.w1b_d[li_, e, h * 512:(h + 1) * 512, :], k.w1[li_, e, h * 512:(h + 1) * 512, :], extra_reads=er)
                S.dma('pool', k.w2b_d[li_, e], k.w2[li_, e], extra_reads=er)
        k.conv_step = conv_step
        for l in layers:
            k.l = l
            k.modT = k.modT_l[l]
            k.need_ctx = (l < DEPTH - 1)
            k.t0 = 0 if k.need_ctx else 2
            if phases == 'ALL' or '1' in phases:
                phase_ln1(k)
            if phases == 'ALL' or 'A' in phases:
                phase_A(k)
            if phases == 'ALL' or 'B' in phases:
                phase_B(k)
            if phases == 'ALL' or 'C' in phases:
                phase_C(k)
            if phases == 'ALL' or 'D' in phases:
                phase_D(k)
            if l == layers[0]:
                dbg_copy('cat', k.cat_d)
            if phases == 'ALL' or 'O' in phases:
                phase_O(k)
            if l == layers[0]:
                dbg_copy('x1', k.xs_d)
                dbg_copy('h2T', k.hT_d)
            if phases == 'ALL' or 'M' in phases:
                phase_M(k)
            if l == layers[0]:
                dbg_copy('xout', k.xs_d)
        S.emit()
    return nc, k


def x_src(k, i):
    if k.l == 0:
        if i < 2:
            return k.ctx[i * 128:(i + 1) * 128, :]
        return k.x[(i - 2) * 128:(i - 1) * 128, :]
    return k.xs_d[i * 128:(i + 1) * 128, :]


def mcol(k, i):
    return 1 if i < 2 else 0


class Ring:
    def __init__(self, es, nc, name, shape, dt, n, psum=False):
        self.b = []
        for j in range(n):
            if psum:
                self.b.append(es.enter_context(nc.psum_tensor(UN(f'{name}{j}'), list(shape), dt)).ap())
            else:
                self.b.append(es.enter_context(nc.sbuf_tensor(UN(f'{name}{j}'), list(shape), dt)).ap())
        self.i = 0

    def next(self):
        r = self.b[self.i % len(self.b)]
        self.i += 1
        return r


def rsqrt_act(k, dst, src, tmp, scale, eps):
    k.S.act(tmp, src, AF.Ln, bias=k.epsb if eps == EPS else eps, scale=scale)
    k.S.act(dst, tmp, AF.Exp, scale=-0.5)


def ln_stats(k, src, n, st, eps=EPS):
    S = k.S
    nch = max(1, n // 512)
    w = n // nch
    for c in range(nch):
        S.op('dve', lambda e, c=c: e.bn_stats(st['s6'][:, c, :], src[:, c * w:(c + 1) * w]),
             [src[:, c * w:(c + 1) * w]], [st['s6'][:, c, :]])
    S.op('dve', lambda e: e.bn_aggr(st['mv'], st['s6'][:, 0:nch, :]), [st['s6'][:, 0:nch, :]], [st['mv']])
    rsqrt_act(k, st['rstd'], st['mv'][:, 1:2], st['ve'], 1.0, eps)
    S.stt('dve', st['nmr'], st['mv'][:, 0:1], -1.0, st['rstd'], ALU.mult, ALU.mult)


def stat_tiles(es, nc, name, n=2):
    out = []
    for j in range(n):
        d = {}
        for nm, shp in (('s6', [128, 2, 6]), ('mv', [128, 2]), ('ve', [128, 1]), ('rstd', [128, 1]), ('nmr', [128, 1])):
            d[nm] = es.enter_context(nc.sbuf_tensor(UN(f'{name}_{nm}{j}'), shp, F32)).ap()
        out.append(d)
    return out


def phase_mod(k):
    nc, S, l = k.nc, k.S, k.l
    with ExitStack() as es:
        sb = lambda n, s, d: es.enter_context(nc.sbuf_tensor(UN(n), list(s), d)).ap()
        cv = sb('m_cv', [128, 8, 2], F32)
        scv = sb('m_scv', [128, 8, 2], F32)
        sig = sb('m_sig', [128, 8, 2], F32)
        adab = sb('m_adab', [2, 6 * D], F32)
        modrow = sb('m_modrow', [2, 6 * D], F32)
        sel = sb('m_sel', [2, 2, 128], F32)
        aw = Ring(es, nc, 'm_aw', [128, 8, 512], F32, 2)
        pm = Ring(es, nc, 'm_pm', [128, 512], F32, 2, psum=True)
        pt = es.enter_context(nc.psum_tensor(UN('m_pt'), [128, 512], F32)).ap()
        for r in range(2):
            S.dma('sp', cv[:, :, r], k.cvec[r, :].rearrange("(kk p) -> p kk", p=128), allow_slow_non_contiguous=True)
        S.dma('sp', adab, k.ada_b[l:l + 1, :].to_broadcast([2, 6 * D]))
        S.dma('sp', sel, k.c_sel)
        S.act(sig, cv, AF.Sigmoid)
        S.tt('dve', scv, cv, sig, ALU.mult)
        for nb in range(12):
            a = aw.next()
            S.dma('sp', a, k.ada_w[l, :, nb * 512:(nb + 1) * 512].rearrange("(kk p) n -> p kk n", p=128))
            p = pm.next()
            for kk in range(8):
                S.mm(p[0:2, :], scv[:, kk, :], a[:, kk, :], start=(kk == 0), stop=(kk == 7))
            S.tt('dve', modrow[:, nb * 512:(nb + 1) * 512], p[0:2, :], adab[:, nb * 512:(nb + 1) * 512], ALU.add)
        ptv = pt[:, 0:96].rearrange("p (j r) -> p j r", r=2)
        for j in range(48):
            S.tr(ptv[:, j, :], modrow[0:2, j * 128:(j + 1) * 128], k.ident_f[0:2, 0:2])
        S.copy('dve', k.modT, ptv)
        for c0 in (8, 32):
            S.ts('dve', k.modT[:, c0:c0 + 8, :], k.modT[:, c0:c0 + 8, :], 1.0, ALU.add)
        if not hasattr(k, 'gate_d'):
            k.gate_d = nc.dram_tensor('gate_d', [DEPTH, 2, 2, D], F32, kind="Internal").ap()
        S.dma('sp', k.gate_d[l, :, 0, :], modrow[:, 2 * D:3 * D])
        S.dma('sp', k.gate_d[l, :, 1, :], modrow[:, 5 * D:6 * D])
    S.barrier()


def emit_hT(k, xn, i, sc0, sh0, pT_ring, hT_ring, dt_is_f32=False):
    S = k.S
    r = mcol(k, i)
    pT = pT_ring.next()
    pTv = pT.rearrange("p (kk t) -> p kk t", kk=8)
    for kk in range(8):
        S.tr(pTv[:, kk, :], xn[:, kk * 128:(kk + 1) * 128], k.ident_b)
    hT = hT_ring.next()
    for kk in range(8):
        sc = k.modT[:, sc0 + kk, r:r + 1]
        sh = k.modT[:, sh0 + kk, r:r + 1]
        if kk % 2 == 0:
            S.act(hT[:, kk, :], pTv[:, kk, :], AF.Identity, bias=sh, scale=sc)
        else:
            S.ts('dve', hT[:, kk, :], pTv[:, kk, :], sc, ALU.mult, sh, ALU.add)
    S.dma('sp', k.hT_d[i], hT)
    return hT


def phase_ln1(k, after=None):
    nc, S = k.nc, k.S
    with ExitStack() as es:
        xt = Ring(es, nc, 'l1_x', [128, D], F32, 2)
        xn = Ring(es, nc, 'l1_xn', [128, D], BF16, 2)
        hT = Ring(es, nc, 'l1_hT', [128, 8, 128], BF16, 2)
        pT = Ring(es, nc, 'l1_pT', [128, 1024], BF16, 2, psum=True)
        sts = stat_tiles(es, nc, 'l1')
        for i in range(NT):
            x = xt.next()
            S.dma('sp', x, x_src(k, i))
            st = sts[i % 2]
            ln_stats(k, x, D, st)
            n = xn.next()
            S.act(n, x, AF.Identity, bias=st['nmr'], scale=st['rstd'])
            emit_hT(k, n, i, 8, 0, pT, hT)
        if after is not None:
            after()
    S.barrier()
    if k.l == k.cfg.get('layers', [0, 1])[0]:
        k.dbg_copy('hT', k.hT_d)


def load_w(k, dst, src_cols, stage):
    c0, c1 = src_cols
    n = c1 - c0
    j = 0
    for a in range(0, n, 256):
        b = min(n, a + 256)
        st = stage.next()
        k.S.dma('sp', st[:, :, 0:b - a], k.w_in[k.l, :, c0 + a:c0 + b].rearrange("(kk p) n -> p kk n", p=128))
        k.S.copy('act' if j % 2 else 'dve', dst[:, :, a:b], st[:, :, 0:b - a])
        j += 1


def wstage(es, nc, name):
    return Ring(es, nc, name, [128, 8, 256], F32, 2)


def proj(k, out_ps, hT, w, n0, n1):
    for kk in range(8):
        k.S.mm(out_ps, hT[:, kk, :], w[:, kk, n0:n1], start=(kk == 0), stop=(kk == 7))


def phase_A(k):
    nc, S, l = k.nc, k.S, k.l
    with ExitStack() as es:
        sb = lambda n, s, d: es.enter_context(nc.sbuf_tensor(UN(n), list(s), d)).ap()
        wA = sb('a_w', [128, 8, 512], BF16)
        stg = wstage(es, nc, 'a_stg')
        load_w(k, wA, (0, 512), stg)
        ws = sb('a_ws', [128, 4, 128], F32)
        wsT = sb('a_wsT', [128, 4, 128], BF16)
        bs4 = sb('a_bs4', [128, 4], F32)
        BS = sb('a_BS', [128, 4, 64], F32)
        LW = sb('a_LW', [128, 256], F32)
        LB = sb('a_LB', [128, 256], F32)
        S.dma('sp', ws, k.sgu_w[l].rearrange("g t s -> t g s"))
        S.dma('sp', bs4, k.sgu_b[l].rearrange("g t -> t g"), allow_slow_non_contiguous=True)
        S.dma('sp', LW, k.sgu_ln_w[l:l + 1, :].to_broadcast([128, 256]))
        S.dma('sp', LB, k.sgu_ln_b[l:l + 1, :].to_broadcast([128, 256]))
        pw = es.enter_context(nc.psum_tensor(UN('a_pw'), [128, 512], F32)).ap()
        pwv = pw.rearrange("p (g t) -> p g t", g=4)
        for g in range(4):
            S.tr(pwv[:, g, :], ws[:, g, :], k.ident_f)
        S.copy('dve', wsT, pwv)
        S.copy('dve', BS, bs4.unsqueeze(2).to_broadcast([128, 4, 64]))
        hTr = Ring(es, nc, 'a_hT', [128, 8, 128], BF16, 2)
        pA = Ring(es, nc, 'a_pA', [128, 512], F32, 2, psum=True)
        pM = Ring(es, nc, 'a_pM', [128, 512], F32, 2, psum=True)
        uvg = Ring(es, nc, 'a_uvg', [128, 512], F32, 2)
        vn = Ring(es, nc, 'a_vn', [128, 256], F32, 2)
        vn2 = Ring(es, nc, 'a_vn2', [128, 256], F32, 2)
        vnb = Ring(es, nc, 'a_vnb', [128, 256], BF16, 2)
        t1 = Ring(es, nc, 'a_t1', [128, 256], F32, 2)
        oa = Ring(es, nc, 'a_oa', [128, 256], BF16, 2)
        sts = stat_tiles(es, nc, 'a')
        def stage_a(i):
            hT = hTr.next()
            S.dma('sp', hT, k.hT_d[i])
            p = pA.next()
            proj(k, p, hT, wA, 0, 512)
            u = uvg.next()
            S.act(u, p, AF.Gelu)
            return u

        def stage_b(i, u):
            st = sts[i % 2]
            ln_stats(k, u[:, 256:512], 256, st)
            v = vn.next()
            S.act(v, u[:, 256:512], AF.Identity, bias=st['nmr'], scale=st['rstd'])
            v2 = vn2.next()
            S.tt('dve', v2, v, LW, ALU.mult)
            vb = vnb.next()
            S.tt('dve', vb, v2, LB, ALU.add)
            pm = pM.next()
            for g in range(4):
                S.mm(pm[:, g * 64:(g + 1) * 64], wsT[:, g, :], vb[:, g * 64:(g + 1) * 64], start=True, stop=True)
            tt1 = t1.next()
            S.tt('dve', tt1, pm[:, 0:256], BS.rearrange("p g c -> p (g c)"), ALU.add)
            o = oa.next()
            S.tt('dve', o, tt1, u[:, 0:256], ALU.mult)
            S.dma('sp', k.cat_d[i * 128:(i + 1) * 128, 0:256], o)
        tl = list(range(k.t0, NT))
        nxt = stage_a(tl[0])
        for n, i in enumerate(tl):
            cur = nxt
            if n + 1 < len(tl):
                nxt = stage_a(tl[n + 1])
            stage_b(i, cur)
    S.barrier()


def rope_apply(k, src5, dst5, cs, G, tmp):
    S = k.S
    cos = cs[:, 0, :].rearrange("p (a f) -> p a f", a=2).unsqueeze(1).to_broadcast([128, G, 2, 8])
    sin = cs[:, 1, :].rearrange("p (a f) -> p a f", a=2).unsqueeze(1).to_broadcast([128, G, 2, 8])
    x1 = src5[:, :, :, 0, :]
    x2 = src5[:, :, :, 1, :]
    ta, tb, tc, td = [t[:, 0:G, :, :] for t in tmp]
    S.tt('dve', ta, x1, cos, ALU.mult)
    S.tt('dve', tb, x2, sin, ALU.mult)
    S.tt('dve', dst5[:, :, :, 0, :], ta, tb, ALU.subtract)
    S.tt('dve', tc, x2, cos, ALU.mult)
    S.tt('dve', td, x1, sin, ALU.mult)
    S.tt('dve', dst5[:, :, :, 1, :], tc, td, ALU.add)


def attention_res(k, es, npairs):
    nc = k.nc
    pS = Ring(es, nc, 'at_pS', [128, 512], F32, 4, psum=True)
    pO = Ring(es, nc, 'at_pO', [128, 512], F32, 2, psum=True)
    pexp = Ring(es, nc, 'at_pe', [128, 512], BF16, 4)
    oTs = Ring(es, nc, 'at_oT', [65, npairs, 512], F32, 2)
    return (pS, pO, pexp, oTs)


def attention_core(k, es, qT, kT, v1, nk_part, scale, pairs, finalize, LA=3):
    nc, S = k.nc, k.S
    pS, pO, pexp, oTs = es
    blocks = []
    if k.need_ctx:
        blocks.append((0, 256, [0, 1]))
    for j in range(8):
        blocks.append((256 + j * 512, 512, list(range(NT))))
    steps = []
    for (q0, qn, kts) in blocks:
        for pi in range(len(pairs)):
            for n, kt in enumerate(kts):
                steps.append((q0, qn, pi, n, kt, len(kts)))

    def issue_score(st):
        q0, qn, pi, n, kt, nk = st
        g, pb, hh = pairs[pi]
        ps = pS.next()
        if callable(g):
            ka, qa = g(kt, q0, qn)
            S.mm(ps[:, 0:qn], ka, qa)
        else:
            S.mm(ps[:, 0:qn], kT[pb:pb + nk_part, g, kt * 128:(kt + 1) * 128], qT[pb:pb + nk_part, g, q0:q0 + qn])
        return ps
    inflight = [issue_score(st) for st in steps[0:LA]]
    po = None
    o_sb = None
    for si, st in enumerate(steps):
        q0, qn, pi, n, kt, nk = st
        g, pb, hh = pairs[pi]
        if si + LA < len(steps):
            inflight.append(issue_score(steps[si + LA]))
        ps = inflight.pop(0)
        if n == 0:
            po = pO.next()
            if pi == 0:
                o_sb = oTs.next()
        pe = pexp.next()
        S.act(pe[:, 0:qn], ps[:, 0:qn], AF.Exp, scale=scale)
        S.mm(po[0:65, 0:qn], v1[:, kt, hh, :], pe[:, 0:qn], start=(n == 0), stop=(n == nk - 1))
        if n == nk - 1:
            S.copy('dve', o_sb[:, pi, 0:qn], po[0:65, 0:qn])
            if pi == len(pairs) - 1:
                for j in range(qn // 128):
                    finalize((q0 // 128) + j, o_sb, j)


def phase_B(k):
    nc, S, l = k.nc, k.S, k.l
    lam_init = 0.8 - 0.6 * math.exp(-0.3 * l)
    scale = 32 ** -0.5
    for hp in range(2):
        with ExitStack() as es:
            sb = lambda n, s, d: es.enter_context(nc.sbuf_tensor(UN(n), list(s), d)).ap()
            wB = sb('b_w', [128, 8, 384], BF16)
            stg = wstage(es, nc, 'b_stg')
            for j, c0 in enumerate((CBQ, CBK)):
                st = stg.next()
                S.dma('sp', st[:, :, 0:128],
                      k.w_in[l, :, c0 + hp * 128:c0 + hp * 128 + 128].rearrange("(kk p) n -> p kk n", p=128))
                sv = st[:, :, 0:128].rearrange("p kk (hh m d) -> p kk hh m d", hh=2, m=2)
                for m in range(2):
                    S.copy('dve' if m else 'act',
                           wB[:, :, j * 128 + m * 64:j * 128 + (m + 1) * 64].rearrange("p kk (hh d) -> p kk hh d", hh=2),
                           sv[:, :, :, m, :])
            load_w(k, wB[:, :, 256:384], (CBV + hp * 128, CBV + hp * 128 + 128), stg)
            qT = sb('b_qT', [128, NTOK], BF16)
            kT = sb('b_kTz', [128, 4, NTOK], BF16)
            rm = sb('b_rm', [128, 4], F32)
            S.dma('sp', rm, k.c_cind)
            v1 = sb('b_v1', [128, NT, 2, 65], BF16)
            oball = sb('b_ob', [128, NT, 2, 64], BF16)
            S.memset('dve', v1[:, :, :, 64:65], 1.0)
            lamt = sb('b_lam', [128, 4, 32], F32)
            lp = sb('b_lp', [128, 2, 32], F32)
            ls = sb('b_ls', [128, 2], F32)
            le = sb('b_le', [128, 2], F32)
            nlam = sb('b_nlam', [128, 1], F32)
            SUBW = sb('b_subw', [128, 64], F32)
            S.dma('sp', lamt, k.diff_lambda[l:l + 1].to_broadcast([128, 4, 32]))
            S.dma('sp', SUBW, k.diff_subln_w[l:l + 1, :].to_broadcast([128, 64]))
            S.ts('dve', SUBW, SUBW, 1.0 - lam_init, ALU.mult)
            lv = lamt.rearrange("p (a b) d -> p a b d", b=2)
            S.tt('dve', lp, lv[:, :, 0, :], lv[:, :, 1, :], ALU.mult)
            S.op('dve', lambda e: e.tensor_reduce(ls, lp, AX.X, ALU.add), [lp], [ls])
            S.act(le, ls, AF.Exp)
            S.tt('dve', nlam, le[:, 1:2], le[:, 0:1], ALU.subtract)
            S.ts('dve', nlam, nlam, -lam_init, ALU.add)
            with ExitStack() as es1:
                hTr = Ring(es1, nc, 'b_hT', [128, 8, 128], BF16, 2)
                pB = Ring(es1, nc, 'b_pB', [128, 512], F32, 2, psum=True)
                pT = Ring(es1, nc, 'b_pT', [128, 1024], BF16, 2, psum=True)
                csr = Ring(es1, nc, 'b_cs', [128, 2, 16], F32, 2)
                qkr = Ring(es1, nc, 'b_qkr', [128, 256], BF16, 2)
                tmp = [es1.enter_context(nc.sbuf_tensor(UN(f'b_tmp{j}'), [128, 8, 2, 8], F32)).ap() for j in range(4)]
                def b_stage_a(i):
                    hT = hTr.next()
                    S.dma('sp', hT, k.hT_d[i])
                    p = pB.next()
                    proj(k, p[:, 0:384], hT, wB, 0, 384)
                    return p
                p_next = b_stage_a(0)
                for i in range(NT):
                    p = p_next
                    if i + 1 < NT:
                        p_next = b_stage_a(i + 1)
                    qk = qkr.next()
                    if i >= 2:
                        cs = csr.next()
                        S.dma('sp', cs, k.c_rope[(i - 2) * 128:(i - 1) * 128])
                        src5 = p[:, 0:256].rearrange("p (g a h f) -> p g a h f", g=8, a=2, h=2)
                        dst5 = qk.rearrange("p (g a h f) -> p g a h f", g=8, a=2, h=2)
                        rope_apply(k, src5, dst5, cs, 8, tmp)
                    else:
                        S.copy('act', qk, p[:, 0:256])
                    S.copy('act', v1[:, i, :, 0:64], p[:, 256:384].rearrange("p (h c) -> p h c", h=2))
                    pt = pT.next()
                    ptv = pt[:, 0:256].rearrange("p (w t) -> p w t", w=2)
                    for w in range(2):
                        S.tr(ptv[:, w, :], qk[:, w * 128:(w + 1) * 128], k.ident_b)
                    S.copy('dve', qT[:, i * 128:(i + 1) * 128], ptv[:, 0, :])
                    for j in range(4):
                        if j % 2 == 0:
                            S.act(kT[:, j, i * 128:(i + 1) * 128], ptv[:, 1, :], AF.Identity, scale=rm[:, j:j + 1])
                        else:
                            S.ts('dve', kT[:, j, i * 128:(i + 1) * 128], ptv[:, 1, :], rm[:, j:j + 1], ALU.mult)
            S.barrier()
            with ExitStack() as es2:
                sb2 = lambda n, s, d: es2.enter_context(nc.sbuf_tensor(UN(n), list(s), d)).ap()
                pF = Ring(es2, nc, 'b_pF', [128, 512], F32, 2, psum=True)
                rden = Ring(es2, nc, 'b_rden', [128, 2], F32, 2)
                a0r = Ring(es2, nc, 'b_a0', [128, 64], F32, 2)
                ar = Ring(es2, nc, 'b_a', [128, 64], F32, 2)
                sqr = Ring(es2, nc, 'b_sq', [128, 64], F32, 2)
                ssr = Ring(es2, nc, 'b_ss', [128, 1], F32, 2)
                rsr = Ring(es2, nc, 'b_rs', [128, 1], F32, 2)
                nl1 = Ring(es2, nc, 'b_nl1', [128, 1], F32, 2)
                ares = attention_res(k, es2, 2)
                for hh in range(2):
                    def fin(ti, o_sb, j, hh=hh):
                        pf = pF.next()
                        pfv = pf[:, 0:256].rearrange("p (m c) -> p m c", m=2)
                        for m in range(2):
                            S.tr(pfv[:, m, 0:65], o_sb[0:65, m, j * 128:(j + 1) * 128], k.ident_f[0:65, 0:65])
                        rd = rden.next()
                        S.op('dve', lambda e: e.reciprocal(rd, pfv[:, :, 64]), [pfv[:, :, 64]], [rd])
                        a0 = a0r.next()
                        S.ts('dve', a0, pfv[:, 0, 0:64], rd[:, 0:1], ALU.mult)
                        n1 = nl1.next()
                        S.tt('dve', n1, rd[:, 1:2], nlam, ALU.mult)
                        a = ar.next()
                        S.stt('dve', a, pfv[:, 1, 0:64], n1, a0, ALU.mult, ALU.add)
                        sq = sqr.next()
                        ss = ssr.next()
                        S.act(sq, a, AF.Square, accum_out=ss)
                        rs = rsr.next()
                        rsqrt_act(k, rs, ss, ss, 1.0 / 64.0, EPS)
                        S.stt('dve', oball[:, ti, hh, :], a, rs, SUBW, ALU.mult, ALU.mult)
                        if ti >= 2 and (ti - 2) % 4 == 3:
                            k.conv_step(oball[0:1, ti, hh, 0:1])
                    def getter(m, hh=hh):
                        return lambda kt, q0, qn: (kT[:, m * 2 + hh, kt * 128:(kt + 1) * 128], qT[:, q0:q0 + qn])
                    attention_core(k, ares, qT, kT, v1, 128, scale, [(getter(0), 0, hh), (getter(1), 0, hh)], fin)
                for i in range(k.t0, NT):
                    S.dma('sp', k.cat_d[i * 128:(i + 1) * 128, 256 + hp * 128:256 + (hp + 1) * 128],
                          oball[:, i, :, :].rearrange("p h c -> p (h c)"))
        S.barrier()


def phase_D(k):
    nc, S, l = k.nc, k.S, k.l
    scale = 96 ** -0.5
    for hp in range(2):
        with ExitStack() as es:
            sb = lambda n, s, d: es.enter_context(nc.sbuf_tensor(UN(n), list(s), d)).ap()
            stg = wstage(es, nc, 'd_stg')
            wD = sb('d_w', [128, 8, 416], BF16)
            load_w(k, wD, (CD0, CD0 + 416), stg)
            wuq_f = sb('d_wuqf', [128, 2, 192], F32)
            S.dma('sp', wuq_f, k.mla_w_uq[l, :, hp * 192:(hp + 1) * 192].rearrange("(c p) n -> p c n", p=128))
            qnw = sb('d_qnw', [128, 2], F32)
            S.dma('sp', qnw, k.mla_q_norm_w[l].rearrange("(c p) -> p c", p=128), allow_slow_non_contiguous=True)
            wuq = sb('d_wuq', [128, 2, 192], BF16)
            for c in range(2):
                S.ts('dve', wuq[:, c, :], wuq_f[:, c, :], qnw[:, c:c + 1], ALU.mult)
            wukv_f = sb('d_wukvf', [128, 256], F32)
            S.dma('sp', wukv_f, k.mla_w_ukv[l, :, hp * 256:(hp + 1) * 256])
            kvw = sb('d_kvw', [128, 1], F32)
            S.dma('sp', kvw, k.mla_kv_norm_w[l].rearrange("(p o) -> p o", o=1))
            wukv = sb('d_wukv', [128, 256], BF16)
            S.ts('dve', wukv, wukv_f, kvw, ALU.mult)
            qT = sb('d_qT', [96, 2, NTOK], BF16)
            kT = sb('d_kT', [96, 2, NTOK], BF16)
            v1 = sb('d_v1', [128, NT, 2, 65], BF16)
            oball = sb('d_ob', [128, NT, 2, 64], BF16)
            S.memset('dve', v1[:, :, :, 64:65], 1.0)
            with ExitStack() as es1:
                sb1 = lambda n, s, d: es1.enter_context(nc.sbuf_tensor(UN(n), list(s), d)).ap()
                hTr = Ring(es1, nc, 'd_hT', [128, 8, 128], BF16, 2)
                pD = Ring(es1, nc, 'd_pD', [128, 512], F32, 2, psum=True)
                pQ = Ring(es1, nc, 'd_pQ', [128, 512], F32, 2, psum=True)
                pT = Ring(es1, nc, 'd_pT', [128, 1024], BF16, 2, psum=True)
                csr = Ring(es1, nc, 'd_cs', [128, 2, 16], F32, 2)
                junk = Ring(es1, nc, 'd_junk', [128, 256], F32, 2)
                ssr = Ring(es1, nc, 'd_ss', [128, 2], F32, 2)
                lnr = Ring(es1, nc, 'd_ln', [128, 2], F32, 2)
                rrr = Ring(es1, nc, 'd_rr', [128, 2], F32, 2)
                cbr = Ring(es1, nc, 'd_cb', [128, 384], BF16, 2)
                cTr = Ring(es1, nc, 'd_cT', [128, 3, 128], BF16, 2)
                qfr = Ring(es1, nc, 'd_qf', [128, 2, 96], BF16, 2)
                kfr = Ring(es1, nc, 'd_kf', [128, 2, 96], BF16, 2)
                qrr = Ring(es1, nc, 'd_qr', [128, 2, 32], F32, 2)
                tmp = [sb1(f'd_tmp{j}', [128, 2, 2, 8], F32) for j in range(4)]
                def d_stage_a(i):
                    hT = hTr.next()
                    S.dma('sp', hT, k.hT_d[i])
                    p = pD.next()
                    proj(k, p[:, 0:416], hT, wD, 0, 416)
                    return p
                p_next = d_stage_a(0)
                for i in range(NT):
                    p = p_next
                    if i + 1 < NT:
                        p_next = d_stage_a(i + 1)
                    jk = junk.next()
                    ss = ssr.next()
                    S.act(jk[:, 0:256], p[:, 0:256], AF.Square, accum_out=ss[:, 0:1])
                    S.act(jk[:, 0:128], p[:, 256:384], AF.Square, accum_out=ss[:, 1:2])
                    ln = lnr.next()
                    rr = rrr.next()
                    rsqrt_act(k, rr[:, 0:1], ss[:, 0:1], ln[:, 0:1], 1.0 / 256.0, EPS)
                    rsqrt_act(k, rr[:, 1:2], ss[:, 1:2], ln[:, 1:2], 1.0 / 128.0, EPS)
                    cb = cbr.next()
                    S.copy('act', cb, p[:, 0:384])
                    pt = pT.next()
                    ptv = pt[:, 0:384].rearrange("p (c t) -> p c t", c=3)
                    for c in range(3):
                        S.tr(ptv[:, c, :], cb[:, c * 128:(c + 1) * 128], k.ident_b)
                    cT = cTr.next()
                    S.copy('dve', cT, ptv)
                    pq = pQ.next()
                    S.mm(pq[:, 0:192], cT[:, 0, :], wuq[:, 0, :], start=True, stop=False)
                    S.mm(pq[:, 0:192], cT[:, 1, :], wuq[:, 1, :], start=False, stop=True)
                    S.mm(pq[:, 256:512], cT[:, 2, :], wukv, start=True, stop=True)
                    pqv = pq[:, 0:192].rearrange("p (h c) -> p h c", h=2)
                    pkv = pq[:, 256:512].rearrange("p (h c) -> p h c", h=2)
                    qf = qfr.next()
                    kf = kfr.next()
                    rq = rr[:, 0:1]
                    rkv = rr[:, 1:2]
                    S.ts('dve', qf[:, :, 0:64], pqv[:, :, 0:64], rq, ALU.mult)
                    S.ts('dve', kf[:, :, 0:64], pkv[:, :, 0:64], rkv, ALU.mult)
                    S.act(v1[:, i, :, 0:64], pkv[:, :, 64:128], AF.Identity, scale=rkv)
                    if i >= 2:
                        cs = csr.next()
                        S.dma('sp', cs, k.c_rope[(i - 2) * 128:(i - 1) * 128])
                        qr = qrr.next()
                        S.ts('dve', qr, pqv[:, :, 64:96], rq, ALU.mult)
                        rope_apply(k, qr.rearrange("p g (a h f) -> p g a h f", a=2, h=2),
                                   qf[:, :, 64:96].rearrange("p g (a h f) -> p g a h f", a=2, h=2), cs, 2, tmp)
                        rope_apply(k, p[:, 384:416].rearrange("p (g a h f) -> p g a h f", g=1, a=2, h=2),
                                   kf[:, 0:1, 64:96].rearrange("p g (a h f) -> p g a h f", a=2, h=2), cs, 1, tmp)
                        S.copy('dve', kf[:, 1, 64:96], kf[:, 0, 64:96])
                    else:
                        S.ts('dve', qf[:, :, 64:96], pqv[:, :, 64:96], rq, ALU.mult)
                        S.copy('dve', kf[:, :, 64:96], p[:, 384:416].unsqueeze(1).to_broadcast([128, 2, 32]))
                    pt2 = pT.next()
                    pt2v = pt2[0:96, 0:512].rearrange("p (w t) -> p w t", w=4)
                    for hh in range(2):
                        S.tr(pt2v[:, hh, :], qf[:, hh, :], k.ident_b)
                        S.tr(pt2v[:, 2 + hh, :], kf[:, hh, :], k.ident_b)
                    S.copy('act', qT[:, :, i * 128:(i + 1) * 128], pt2v[:, 0:2, :])
                    S.copy('dve', kT[:, :, i * 128:(i + 1) * 128], pt2v[:, 2:4, :])
            S.barrier()
            with ExitStack() as es2:
                pF = Ring(es2, nc, 'd_pF', [128, 512], F32, 2, psum=True)
                rden = Ring(es2, nc, 'd_rden', [128, 1], F32, 2)
                ares = attention_res(k, es2, 1)
                for hh in range(2):
                    def fin(ti, o_sb, j, hh=hh):
                        pf = pF.next()
                        S.tr(pf[:, 0:65], o_sb[0:65, 0, j * 128:(j + 1) * 128], k.ident_f[0:65, 0:65])
                        rd = rden.next()
                        S.op('dve', lambda e: e.reciprocal(rd, pf[:, 64:65]), [pf[:, 64:65]], [rd])
                        S.ts('dve', oball[:, ti, hh, :], pf[:, 0:64], rd, ALU.mult)
                        if ti >= 2 and (ti - 2) % 4 == 3:
                            k.conv_step(oball[0:1, ti, hh, 0:1])
                    attention_core(k, ares, qT, kT, v1, 96, scale, [(hh, 0, hh)], fin)
                for i in range(k.t0, NT):
                    S.dma('sp', k.cat_d[i * 128:(i + 1) * 128, 768 + hp * 128:768 + (hp + 1) * 128],
                          oball[:, i, :, :].rearrange("p h c -> p (h c)"))
        S.barrier()


def phase_C(k):
    nc, S, l = k.nc, k.S, k.l
    with ExitStack() as es:
        sb = lambda n, s, d: es.enter_context(nc.sbuf_tensor(UN(n), list(s), d)).ap()
        stg = wstage(es, nc, 'c_stg')
        wC = sb('c_w', [128, 8, 1280], BF16)
        load_w(k, wC, (CC0, CC0 + 1280), stg)
        gm = sb('c_gm', [128, 4, 128], F32)
        S.dma('sp', gm, k.c_gmask.rearrange("m s t -> s m t"))
        ci = sb('c_ci', [128, 4], F32)
        S.dma('sp', ci, k.c_cind)
        NW = sb('c_NW', [128, 64], F32)
        S.dma('sp', NW, k.hgrn_norm_w[l:l + 1, :].to_broadcast([128, 64]))
        if l > 0:
            LBt = sb('c_LB', [128, 2, 256], F32)
            OML = sb('c_OML', [128, 2, 256], F32)
            h0 = sb('c_h0', [128, 2, 256], F32)
            S.dma('sp', h0, k.hlb[0:1].to_broadcast([128, 2, 256]))
            S.dma('sp', LBt, k.hlb[1:2].to_broadcast([128, 2, 256]))
            S.tt('dve', h0, LBt, h0, ALU.subtract)
            S.act(LBt, h0, AF.Sigmoid)
            S.ts('dve', OML, LBt, -1.0, ALU.mult, 1.0, ALU.add)
        S_st = sb('c_S', [64, 4, 64], F32)
        tmpS = sb('c_tmpS', [64, 4, 64], F32)
        ps = lambda n, dt=F32, w=512: es.enter_context(nc.psum_tensor(UN(n), [128, w], dt)).ap()
        p1, p2, pb, pSc, pdS, poT, pm = [ps(n) for n in ('c_p1', 'c_p2', 'c_pb', 'c_pS', 'c_pdS', 'c_poT', 'c_pm')]
        pT = ps('c_pT', BF16, 1024)
        hTr = Ring(es, nc, 'c_hT', [128, 8, 128], BF16, 2)
        f32t = lambda nm: Ring(es, nc, nm, [128, 256], F32, 2)
        qhr, sgr, fr, kkr, lfr, e1r, e2r, e3r, krfr = [f32t(n) for n in
                                                       ('c_qh', 'c_sg', 'c_f', 'c_kk', 'c_lf', 'c_e1', 'c_e2', 'c_e3', 'c_krf')]
        qdr = Ring(es, nc, 'c_qd', [128, 256], BF16, 2)
        kdr = Ring(es, nc, 'c_kd', [128, 256], BF16, 2)
        kr4r = Ring(es, nc, 'c_kr4', [128, 4, 256], BF16, 2)
        slots = []
        for j in range(2):
            d_ = {}
            d_['qkT'] = sb(f'c_qkT{j}', [64, 8, 128], BF16)
            d_['A'] = sb(f'c_A{j}', [128, 4, 128], BF16)
            d_['dS'] = sb(f'c_dS{j}', [64, 4, 4, 64], F32)
            d_['dec'] = sb(f'c_dec{j}', [64, 4, 4], F32)
            d_['vb'] = sb(f'c_vb{j}', [128, 256], BF16)
            d_['sgl'] = sb(f'c_sgl{j}', [128, 256], F32)
            d_['Sbf4'] = sb(f'c_Sbf4{j}', [64, 4, 4, 64], BF16)
            slots.append(d_)
        oTsr = Ring(es, nc, 'c_oTs', [64, 4, 128], F32, 2)
        oFr = Ring(es, nc, 'c_oF', [64, 4, 128], F32, 2)
        sqr = Ring(es, nc, 'c_sq', [128, 4, 64], F32, 2)
        ofr = Ring(es, nc, 'c_of', [128, 4, 64], F32, 2)
        onr = Ring(es, nc, 'c_on', [128, 4, 64], F32, 2)
        ocr = Ring(es, nc, 'c_oc', [128, 256], BF16, 2)
        ss4r = Ring(es, nc, 'c_ss4', [128, 4], F32, 2)
        ln4r = Ring(es, nc, 'c_ln4', [128, 4], F32, 2)
        rs4r = Ring(es, nc, 'c_rs4', [128, 4], F32, 2)

        def prep(i, d, sl):
            mi = 0 if d == 0 else 1
            mx = 2 if d == 0 else 3
            hT = hTr.next()
            S.dma('sp', hT, k.hT_d[i])
            proj(k, p1, hT, wC, 0, 512)
            proj(k, p2[:, 0:256], hT, wC, 512 + 256 * d, 768 + 256 * d)
            if d == 1:
                proj(k, p2[:, 256:512], hT, wC, 1024, 1280)
            qh = qhr.next()
            S.act(qh, p1[:, 0:256], AF.Silu)
            S.copy('dve', sl['vb'], p1[:, 256:512])
            sg = sgr.next()
            S.act(sg, p2[:, 0:256], AF.Sigmoid)
            if d == 1:
                S.act(sl['sgl'], p2[:, 256:512], AF.Silu)
            if l > 0:
                f = fr.next()
                S.tt('dve', f, sg, OML[:, d, :], ALU.mult)
                S.tt('dve', f, f, LBt[:, d, :], ALU.add)
            else:
                f = sg
            kk_ = kkr.next()
            S.ts('dve', kk_, f, -1.0, ALU.mult, 1.0, ALU.add)
            lf = lfr.next()
            S.act(lf, f, AF.Ln)
            S.mm(pb[:, 0:256], gm[:, mi, :], lf)
            S.mm(pb[:, 256:512], gm[:, mx, :], lf)
            e1, e2, e3 = e1r.next(), e2r.next(), e3r.next()
            S.act(e1, pb[:, 0:256], AF.Exp)
            S.act(e2, pb[:, 0:256], AF.Exp, scale=-1.0)
            S.act(e3, pb[:, 256:512], AF.Exp)
            qd, kd, krf, kr4 = qdr.next(), kdr.next(), krfr.next(), kr4r.next()
            S.tt('dve', qd, qh, e1, ALU.mult)
            S.tt('dve', kd, kk_, e2, ALU.mult)
            S.tt('dve', krf, kk_, e3, ALU.mult)
            S.tt('dve', kr4, krf.unsqueeze(1).to_broadcast([128, 4, 256]),
                 ci.unsqueeze(2).to_broadcast([128, 4, 256]), ALU.mult)
            for h in range(4):
                S.mm(pm[0:64, 256 + h * 4:260 + h * 4], lf[:, h * 64:(h + 1) * 64], ci)
            S.act(sl['dec'], pm[0:64, 256:272].rearrange("p (h c) -> p h c", h=4), AF.Exp)
            pTv = pT[0:64, :].rearrange("p (w t) -> p w t", w=8)
            for h in range(4):
                S.tr(pTv[:, h, :], qd[:, h * 64:(h + 1) * 64], k.ident_b)
                S.tr(pTv[:, 4 + h, :], kd[:, h * 64:(h + 1) * 64], k.ident_b)
            S.copy('dve', sl['qkT'], pTv)
            for h in range(4):
                S.mm(pSc[:, h * 128:(h + 1) * 128], sl['qkT'][:, 4 + h, :], sl['qkT'][:, h, :])
            S.tt('dve', sl['A'], pSc.rearrange("p (h t) -> p h t", h=4),
                 gm[:, mi, :].unsqueeze(1).to_broadcast([128, 4, 128]), ALU.mult)
            for half in range(2):
                for cc in range(2):
                    c = 2 * half + cc
                    for h in range(4):
                        S.mm(pdS[0:64, (cc * 4 + h) * 64:(cc * 4 + h + 1) * 64], kr4[:, c, h * 64:(h + 1) * 64],
                             sl['vb'][:, h * 64:(h + 1) * 64])
                S.copy('act', sl['dS'][:, 2 * half:2 * half + 2, :, :],
                       pdS[0:64, :].rearrange("p (c h v) -> p c h v", c=2, h=4))

        def chain(i, d, sl):
            for h in range(4):
                S.op('pe', lambda e, h=h: e.matmul(poT[0:64, h * 128:(h + 1) * 128], sl['vb'][:, h * 64:(h + 1) * 64],
                                                   sl['A'][:, h, :], start=(h == 0), stop=False, skip_group_check=True),
                     [sl['vb'][:, h * 64:(h + 1) * 64], sl['A'][:, h, :]], [poT])
            corder = [0, 1, 2, 3] if d == 0 else [3, 2, 1, 0]
            for n, c in enumerate(corder):
                S.copy('dve', sl['Sbf4'][:, c, :, :], S_st)
                S.tt('dve', tmpS, S_st, sl['dec'][:, :, c].unsqueeze(2).to_broadcast([64, 4, 64]), ALU.mult)
                S.tt('dve', S_st, tmpS, sl['dS'][:, c, :, :], ALU.add)
            for n, c in enumerate(corder):
                for h in range(4):
                    S.op('pe', lambda e, h=h, c=c, n=n: e.matmul(
                        poT[0:64, h * 128 + c * 32:h * 128 + (c + 1) * 32], sl['Sbf4'][:, c, h, :],
                        sl['qkT'][:, h, c * 32:(c + 1) * 32],
                        start=False, stop=(n == 3 and h == 3), skip_group_check=True),
                        [sl['Sbf4'][:, c, h, :], sl['qkT'][:, h, c * 32:(c + 1) * 32]], [poT])
            oTs = oTsr.next()
            S.copy('act', oTs, poT[0:64, :].rearrange("p (h t) -> p h t", h=4))
            if d == 0:
                S.dma('sp', k.oF_d[i], oTs)
                return
            if i < k.t0:
                return
            oF = oFr.next()
            S.dma('sp', oF, k.oF_d[i])
            S.tt('dve', oTs, oTs, oF, ALU.add)
            pmv = pm[:, 0:256].rearrange("p (h v) -> p h v", h=4)
            for h in range(4):
                S.tr(pmv[:, h, :], oTs[:, h, :], k.ident_f[0:64, 0:64])
            of = ofr.next()
            S.copy('act', of, pmv)
            pmv = of
            sq = sqr.next()
            S.tt('dve', sq, pmv, pmv, ALU.mult)
            ss4 = ss4r.next()
            S.op('dve', lambda e: e.tensor_reduce(ss4, sq, AX.X, ALU.add), [sq], [ss4])
            rs4 = rs4r.next()
            rsqrt_act(k, rs4, ss4, ln4r.next(), 1.0 / 64.0, EPS)
            on = onr.next()
            S.tt('dve', on, pmv, rs4.unsqueeze(2).to_broadcast([128, 4, 64]), ALU.mult)
            S.tt('dve', on, on, NW.unsqueeze(1).to_broadcast([128, 4, 64]), ALU.mult)
            oc = ocr.next()
            S.tt('dve', oc, on.rearrange("p h v -> p (h v)"), sl['sgl'], ALU.mult)
            S.dma('sp', k.cat_d[i * 128:(i + 1) * 128, 512:768], oc)

        for d in range(2):
            order = list(range(NT)) if d == 0 else [1, 0] + list(range(NT - 1, 1, -1))
            S.memset('dve', S_st, 0.0)
            prep(order[0], d, slots[0])
            for n, i in enumerate(order):
                if n + 1 < len(order):
                    prep(order[n + 1], d, slots[(n + 1) % 2])
                chain(i, d, slots[n % 2])
    S.barrier()


def load_w_generic(k, dst, src, stage):
    n = src.shape[1]
    j = 0
    for a in range(0, n, 256):
        b = min(n, a + 256)
        st = stage.next()
        k.S.dma('sp', st[:, :, 0:b - a], src[:, a:b].rearrange("(kk p) n -> p kk n", p=128))
        k.S.copy('act' if j % 2 else 'dve', dst[:, :, a:b], st[:, :, 0:b - a])
        j += 1


def residual_ln(k, y_halves, gate_t, x_tile, lw, lb, out_t, z, zn, st):
    S = k.S
    for cb in range(2):
        S.tt('dve', z[:, cb * 512:(cb + 1) * 512], y_halves[cb], gate_t[:, cb * 512:(cb + 1) * 512], ALU.mult)
    S.stt('dve', z, x_tile, ALPHA, z, ALU.mult, ALU.add)
    ln_stats(k, z, D, st)
    S.act(zn, z, AF.Identity, bias=st['nmr'], scale=st['rstd'])
    S.tt('dve', zn, zn, lw, ALU.mult)
    S.tt('dve', out_t, zn, lb, ALU.add)


def phase_O(k):
    nc, S, l = k.nc, k.S, k.l
    with ExitStack() as es:
        sb = lambda n, s, d: es.enter_context(nc.sbuf_tensor(UN(n), list(s), d)).ap()
        stg = wstage(es, nc, 'o_stg')
        wO = sb('o_w', [128, 8, 1024], BF16)
        load_w_generic(k, wO, k.w_out[l], stg)
        G1 = sb('o_G1', [128, 2, D], F32)
        for r in range(2):
            S.dma('sp', G1[:, r, :], k.gate_d[l, r:r + 1, 0, :].to_broadcast([128, D]))
        LW = sb('o_LW', [128, D], F32)
        LB = sb('o_LB', [128, D], F32)
        S.dma('sp', LW, k.ln_mix_w[l:l + 1, :].to_broadcast([128, D]))
        S.dma('sp', LB, k.ln_mix_b[l:l + 1, :].to_broadcast([128, D]))
        rw = sb('o_rw', [128, 8, NEXP], F32)
        S.dma('sp', rw, k.router_w[l].rearrange("(kk p) e -> p kk e", p=128))
        RB = sb('o_RB', [128, NEXP], F32)
        S.dma('sp', RB, k.router_b[l:l + 1, :].to_broadcast([128, NEXP]))
        ctr = Ring(es, nc, 'o_ct', [128, D], BF16, 2)
        cTr = Ring(es, nc, 'o_cT', [128, 8, 128], BF16, 2)
        xtr = Ring(es, nc, 'o_xt', [128, D], F32, 2)
        zr = Ring(es, nc, 'o_z', [128, D], F32, 2)
        znr = Ring(es, nc, 'o_zn', [128, D], F32, 2)
        x1r = Ring(es, nc, 'o_x1', [128, D], F32, 2)
        xn2r = Ring(es, nc, 'o_xn2', [128, D], F32, 2)
        h2fr = Ring(es, nc, 'o_h2f', [128, 8, 128], F32, 2)
        h2br = Ring(es, nc, 'o_h2b', [128, 8, 128], BF16, 2)
        pT = Ring(es, nc, 'o_pT', [128, 1024], BF16, 1, psum=True)
        pY = Ring(es, nc, 'o_pY', [128, 512], F32, 4, psum=True)
        pFt = Ring(es, nc, 'o_pF', [128, 512], F32, 2, psum=True)
        pL = Ring(es, nc, 'o_pL', [128, 512], F32, 1, psum=True)
        sts = stat_tiles(es, nc, 'o', 4)
        lgr = Ring(es, nc, 'o_lg', [128, NEXP], F32, 2)
        t8r = Ring(es, nc, 'o_t8', [128, 8], F32, 2)
        mkr = Ring(es, nc, 'o_mk', [128, NEXP], F32, 2)
        exr = Ring(es, nc, 'o_ex', [128, NEXP], F32, 2)
        smr = Ring(es, nc, 'o_sm', [128, 2], F32, 2)
        def stage_a(i):
            ct = ctr.next()
            S.dma('sp', ct, k.cat_d[i * 128:(i + 1) * 128, :])
            pt = pT.next()
            ptv = pt.rearrange("p (kk t) -> p kk t", kk=8)
            for kk in range(8):
                S.tr(ptv[:, kk, :], ct[:, kk * 128:(kk + 1) * 128], k.ident_b)
            cT = cTr.next()
            S.copy('act', cT[:, 0:4, :], ptv[:, 0:4, :])
            S.copy('dve', cT[:, 4:8, :], ptv[:, 4:8, :])
            py = [pY.next(), pY.next()]
            for cb in range(2):
                for kk in range(8):
                    S.mm(py[cb], cT[:, kk, :], wO[:, kk, cb * 512:(cb + 1) * 512], start=(kk == 0), stop=(kk == 7))
            xt = xtr.next()
            S.dma('sp', xt, x_src(k, i))
            return py, xt

        def stage_b(i, py, xt):
            r = mcol(k, i)
            x1 = x1r.next()
            residual_ln(k, py, G1[:, r, :], xt, LW, LB, x1, zr.next(), znr.next(), sts[(2 * i) % 4])
            S.dma('sp', k.xs_d[i * 128:(i + 1) * 128, :], x1)
            st2 = sts[(2 * i + 1) % 4]
            ln_stats(k, x1, D, st2)
            xn2 = xn2r.next()
            S.act(xn2, x1, AF.Identity, bias=st2['nmr'], scale=st2['rstd'])
            pf = [pFt.next(), pFt.next()]
            for kk in range(8):
                S.tr(pf[kk // 4][:, (kk % 4) * 128:(kk % 4 + 1) * 128], xn2[:, kk * 128:(kk + 1) * 128], k.ident_f)
            h2f = h2fr.next()
            for kk in range(8):
                sc = k.modT[:, 32 + kk, r:r + 1]
                sh = k.modT[:, 24 + kk, r:r + 1]
                src = pf[kk // 4][:, (kk % 4) * 128:(kk % 4 + 1) * 128]
                if kk % 2 == 0:
                    S.act(h2f[:, kk, :], src, AF.Identity, bias=sh, scale=sc)
                else:
                    S.ts('dve', h2f[:, kk, :], src, sc, ALU.mult, sh, ALU.add)
            pl = pL.next()
            for kk in range(8):
                S.mm(pl[:, 0:NEXP], h2f[:, kk, :], rw[:, kk, :], start=(kk == 0), stop=(kk == 7))
            h2b = h2br.next()
            S.copy('act', h2b, h2f)
            S.dma('sp', k.hT_d[i], h2b)
            lg = lgr.next()
            S.tt('dve', lg, pl[:, 0:NEXP], RB, ALU.add)
            t8 = t8r.next()
            S.op('dve', lambda e, t8=t8, lg=lg: e.max(t8, lg), [lg], [t8])
            mk = mkr.next()
            S.ts('dve', mk, lg, t8[:, 3:4], ALU.is_ge)
            sm = smr.next()
            S.ts('dve', sm[:, 0:1], t8[:, 0:1], -1.0, ALU.mult)
            ex = exr.next()
            S.act(ex, lg, AF.Exp, bias=sm[:, 0:1])
            S.tt('dve', ex, ex, mk, ALU.mult)
            S.op('dve', lambda e, sm=sm, ex=ex: e.tensor_reduce(sm[:, 1:2], ex, AX.X, ALU.add), [ex], [sm[:, 1:2]])
            S.op('dve', lambda e, sm=sm: e.reciprocal(sm[:, 1:2], sm[:, 1:2]), [sm[:, 1:2]], [sm[:, 1:2]])
            S.ts('dve', k.gates[:, i, :], ex, sm[:, 1:2], ALU.mult)
        tl = list(range(k.t0, NT))
        nxt = stage_a(tl[0])
        for n, i in enumerate(tl):
            cur = nxt
            if n + 1 < len(tl):
                nxt = stage_a(tl[n + 1])
            stage_b(i, cur[0], cur[1])
    S.barrier()


def phase_M(k):
    nc, S, l = k.nc, k.S, k.l
    li = k.cfg.get('layers', [0, 1]).index(l)
    while k.conv_list and k.conv_list[0][0] <= li:
        k.conv_step(None)
    nexp = k.nexp
    tiles = list(range(k.t0, NT))
    GS = 12
    groups = [tiles[a:a + GS] for a in range(0, len(tiles), GS)]
    last = (l == DEPTH - 1)
    with ExitStack() as es:
        sb = lambda n, s, d: es.enter_context(nc.sbuf_tensor(UN(n), list(s), d)).ap()
        pg = Ring(es, nc, 'm_pg', [128, 512], F32, 2, psum=True)
        pl = Ring(es, nc, 'm_pl', [128, 512], F32, 2, psum=True)
        pY = Ring(es, nc, 'm_pY', [128, 512], F32, 2, psum=True)
        pX = Ring(es, nc, 'm_pX', [128, 512], F32, 1, psum=True)
        B1T = sb('m_B1T', [128, 16, nexp], F32)
        with ExitStack() as esb:
            b1raw = esb.enter_context(nc.sbuf_tensor(UN('m_b1raw'), [nexp, 2 * D], F32)).ap()
            S.dma('sp', b1raw, k.b1[li])
            px = pX.next()
            pxv = px[:, 0:16 * nexp].rearrange("p (j e) -> p j e", j=16)
            for fb in range(8):
                for two in range(2):
                    S.tr(pxv[:, fb * 2 + two, :], b1raw[0:nexp, fb * 256 + two:fb * 256 + 256:2], k.ident_f[0:nexp, 0:nexp])
            S.copy('dve', B1T, pxv)
        S.barrier()
        b2f = sb('m_b2f', [nexp, D], F32)
        S.dma('sp', b2f, k.b2[li])
        G2 = sb('m_G2', [128, 2, D], F32)
        for r in range(2):
            S.dma('sp', G2[:, r, :], k.gate_d[l, r:r + 1, 1, :].to_broadcast([128, D]))
        LW = sb('m_LW', [128, D], F32)
        LB = sb('m_LB', [128, D], F32)
        S.dma('sp', LW, k.ln_ffn_w[l:l + 1, :].to_broadcast([128, D]))
        S.dma('sp', LB, k.ln_ffn_b[l:l + 1, :].to_broadcast([128, D]))
        acc = sb('m_acc', [128, GS, D], F32)
        h2g = sb('m_h2g', [128, 8, GS * 128], BF16)
        aT = sb('m_aT', [128, 8, GS * 128], BF16)
        w1r = Ring(es, nc, 'm_w1', [128, 8, 256], BF16, 3)
        w2r = Ring(es, nc, 'm_w2', [128, 8, D], BF16, 2)
        gr = Ring(es, nc, 'm_g', [128, 512], F32, 2)
        sr = Ring(es, nc, 'm_s', [128, 512], F32, 2)
        lr = Ring(es, nc, 'm_l', [128, 512], F32, 2)
        gTr = Ring(es, nc, 'm_gT', [nexp, 128], F32, 2)
        xtr = Ring(es, nc, 'm_xt', [128, D], F32, 1)
        zr = Ring(es, nc, 'm_z', [128, D], F32, 1)
        znr = Ring(es, nc, 'm_zn', [128, D], F32, 1)
        xor_ = Ring(es, nc, 'm_xo', [128, D], F32, 2)
        sts = stat_tiles(es, nc, 'm', 2)
        for grp in groups:
            n = len(grp)
            ntok = n * 128
            for j, i in enumerate(grp):
                S.dma('sp', h2g[:, :, j * 128:(j + 1) * 128], k.hT_d[i])
            for e in range(nexp):
                w2e = w2r.next()
                S.dma('sp', w2e, k.w2b_d[li, e].rearrange("(fc p) d -> p fc d", p=128))
                for fb in range(8):
                    w1s = w1r.next()
                    S.dma('sp', w1s, k.w1b_d[li, e, :, fb * 256:(fb + 1) * 256].rearrange("(kk p) c -> p kk c", p=128))
                    b1g = B1T[:, fb * 2, e:e + 1]
                    b1l = B1T[:, fb * 2 + 1, e:e + 1]
                    for t0 in range(0, ntok, 512):
                        nb = min(512, ntok - t0)
                        a, b = pg.next(), pl.next()
                        for kk in range(8):
                            S.mm(a[:, 0:nb], w1s[:, kk, 0:256:2], h2g[:, kk, t0:t0 + nb], start=(kk == 0), stop=(kk == 7))
                        for kk in range(8):
                            S.mm(b[:, 0:nb], w1s[:, kk, 1:256:2], h2g[:, kk, t0:t0 + nb], start=(kk == 0), stop=(kk == 7))
                        g = gr.next()
                        S.ts('dve', g[:, 0:nb], a[:, 0:nb], b1g, ALU.add, SW_LIM, ALU.min)
                        s_ = sr.next()
                        S.act(s_[:, 0:nb], g[:, 0:nb], AF.Sigmoid, scale=SW_ALPHA)
                        lt = lr.next()
                        S.act(lt[:, 0:nb], b[:, 0:nb], AF.Identity, bias=b1l)
                        S.ts('dve', lt[:, 0:nb], lt[:, 0:nb], SW_LIM, ALU.min, -SW_LIM, ALU.max)
                        S.tt('dve', g[:, 0:nb], g[:, 0:nb], s_[:, 0:nb], ALU.mult)
                        S.stt('dve', aT[:, fb, t0:t0 + nb], lt[:, 0:nb], 1.0, g[:, 0:nb], ALU.add, ALU.mult)
                for j, i in enumerate(grp):
                    gate = k.gates[:, i, e:e + 1]
                    for cb in range(2):
                        py = pY.next()
                        for fb in range(8):
                            S.mm(py, aT[:, fb, j * 128:(j + 1) * 128], w2e[:, fb, cb * 512:(cb + 1) * 512],
                                 start=(fb == 0), stop=(fb == 7))
                        dst = acc[:, j, cb * 512:(cb + 1) * 512]
                        if e == 0:
                            S.ts('dve', dst, py, gate, ALU.mult)
                        else:
                            S.stt('dve', dst, py, gate, dst, ALU.mult, ALU.add)
            for j, i in enumerate(grp):
                r = mcol(k, i)
                px = pX.next()
                S.tr(px[0:nexp, 0:128], k.gates[:, i, 0:nexp], k.ident_f)
                gT = gTr.next()
                S.copy('act', gT, px[0:nexp, 0:128])
                pyb = [pY.next(), pY.next()]
                for cb in range(2):
                    S.mm(pyb[cb], gT, b2f[0:nexp, cb * 512:(cb + 1) * 512])
                    S.tt('dve', acc[:, j, cb * 512:(cb + 1) * 512], acc[:, j, cb * 512:(cb + 1) * 512], pyb[cb], ALU.add)
                xt = xtr.next()
                S.dma('sp', xt, k.xs_d[i * 128:(i + 1) * 128, :])
                xo = xor_.next()
                residual_ln(k, [acc[:, j, 0:512], acc[:, j, 512:1024]], G2[:, r, :], xt, LW, LB, xo, zr.next(), znr.next(),
                            sts[j % 2])
                if last:
                    S.dma('sp', k.y[(i - 2) * 128:(i - 1) * 128, :], xo)
                else:
                    S.dma('sp', k.xs_d[i * 128:(i + 1) * 128, :], xo)
    S.barrier()


_W_KEYS = ['ada_w', 'ada_b', 'w_in', 'w_out', 'sgu_ln_w', 'sgu_ln_b', 'sgu_w', 'sgu_b', 'diff_lambda', 'diff_subln_w',
           'hgrn_lower_bounds', 'hgrn_norm_w', 'mla_q_norm_w', 'mla_w_uq', 'mla_kv_norm_w', 'mla_w_ukv',
           'ln_mix_w', 'ln_mix_b', 'ln_ffn_w', 'ln_ffn_b', 'router_w', 'router_b',
           'expert_w1', 'expert_b1', 'expert_w2', 'expert_b2']


def make_in_maps(inputs, cores, layers=(0, 1)):
    f = lambda a: np.ascontiguousarray(np.asarray(a), dtype=np.float32)
    consts = host_consts()
    shared = {kk: f(inputs[kk]) for kk in _W_KEYS}
    if tuple(layers) != (0, 1):
        for kk in ('expert_w1', 'expert_b1', 'expert_w2', 'expert_b2'):
            shared[kk] = np.ascontiguousarray(shared[kk][list(layers)])
    x = np.asarray(inputs['x'])
    ctx = np.asarray(inputs['ctx'])
    c = np.asarray(inputs['c'])
    cc = np.asarray(inputs['c_ctx'])
    maps = []
    for i in cores:
        m = dict(shared)
        m.update(consts)
        m['x'] = f(x[i])
        m['ctx'] = f(ctx[i])
        m['cvec'] = f(np.stack([c[i], cc], 0))
        maps.append(m)
    return maps


def kernel(**inputs):
    nc, k = build_program({})
    maps = make_in_maps(inputs, range(8))
    res = run_bass_kernel_spmd(nc, maps, core_ids=list(range(8)))
    return np.stack([np.asarray(r['y'], dtype=np.float32) for r in res.results], 0)
```

```python
import math
from contextlib import ExitStack
from concourse.bass_utils import run_bass_kernel_spmd
import numpy as np
import concourse.bass as bass
import concourse.mybir as mybir

F32 = mybir.dt.float32
BF16 = mybir.dt.bfloat16
I32 = mybir.dt.int32
U32 = mybir.dt.uint32
AF = mybir.ActivationFunctionType
ALU = mybir.AluOpType
AX = mybir.AxisListType

_DT_SIZE = {}


def dt_size(dt):
    s = _DT_SIZE.get(dt)
    if s is None:
        n = str(dt)
        if '64' in n:
            s = 8
        elif '32' in n:
            s = 4
        elif '16' in n:
            s = 2
        else:
            s = 1
        _DT_SIZE[dt] = s
    return s


EPOCH = 30000


class Sched:
    ENG = ['pe', 'dve', 'act', 'pool', 'sp']

    def __init__(self, nc, lanes=None):
        self.nc = nc
        self.ops = {e: [] for e in self.ENG}
        self.cnt = {}
        self.seen = {e: {} for e in self.ENG}
        self.wr = {}
        self.rd = {}
        lanes = lanes or {'sp': 8, 'pool': 6, 'act': 2}
        self.lanes = {qn: [f'd_{qn}{i}' for i in range(n)] for qn, n in lanes.items()}
        self.lane_rr = {qn: 0 for qn in lanes}
        self.signal = {}
        self.psum_names = set()

    def box(self, ap):
        t = ap.tensor
        name = t.name
        pat = ap.ap
        off = ap.offset
        sp = type(t).__name__
        if sp.startswith('DRam') or sp.startswith('Dram'):
            ext = 1
            for st, c in pat:
                ext += abs(st) * (c - 1)
            return name, (0, 1, off, off + ext)
        if sp.startswith('PSum'):
            self.psum_names.add(name)
            return name, (0, 128, 0, 1 << 30)
        shp = list(t.shape)
        F = 1
        for d in shp[1:]:
            F *= d
        ts_ = dt_size(t.dtype)
        vs = dt_size(ap.dtype)
        Fv = F * ts_ // vs if vs != ts_ else F
        pst, pc = pat[0]
        if pc > 1:
            assert pst == Fv, (name, pat, Fv)
        p0 = off // Fv
        f0 = off % Fv
        ext = 1
        for st, c in pat[1:]:
            ext += abs(st) * (c - 1)
        return name, (p0, p0 + pc, f0 * vs, (f0 + ext) * vs)

    @staticmethod
    def _ov(a, b):
        return a[0] < b[1] and b[0] < a[1] and a[2] < b[3] and b[2] < a[3]

    @staticmethod
    def _cov(a, b):
        return a[0] <= b[0] and a[1] >= b[1] and a[2] <= b[2] and a[3] >= b[3]

    def _deps(self, reads, writes):
        toks = {}

        def add(k, v):
            if toks.get(k, -1) < v:
                toks[k] = v
        for nm, bx in reads:
            for wb, (k, v) in self.wr.get(nm, ()):
                if self._ov(bx, wb):
                    add(k, v)
        for nm, bx in writes:
            for wb, (k, v) in self.wr.get(nm, ()):
                if self._ov(bx, wb):
                    add(k, v)
            for (rb, k), v in self.rd.get(nm, {}).items():
                if self._ov(bx, rb):
                    add(k, v)
        return toks

    def _record(self, reads, writes, tok):
        k, v = tok
        for nm, bx in reads:
            self.rd.setdefault(nm, {})[(bx, k)] = v
        for nm, bx in writes:
            lst = [(wb, t) for wb, t in self.wr.get(nm, ()) if not self._cov(bx, wb)]
            lst.append((bx, tok))
            self.wr[nm] = lst
            d = self.rd.get(nm)
            if d:
                for key in [key for key in d if self._cov(bx, key[0])]:
                    del d[key]

    def _waits_for(self, eng, toks, skip_key=None):
        out = []
        seen = self.seen[eng]
        for k, v in toks.items():
            if k == skip_key:
                continue
            if seen.get(k, -1) >= v:
                continue
            seen[k] = v
            out.append((k, v))
            self.signal.setdefault(k, set()).add(v)
        return out

    def _boxes(self, reads, writes):
        rb = []
        wb = []
        for a in reads:
            b = self.box(a)
            (wb if b[0] in self.psum_names else rb).append(b)
        for a in writes:
            wb.append(self.box(a))
        return rb, wb

    def op(self, eng, fn, reads=(), writes=()):
        rb, wb = self._boxes(reads, writes)
        toks = self._deps(rb, wb)
        idx = self.cnt.get(eng, 0)
        self.cnt[eng] = idx + 1
        waits = self._waits_for(eng, toks, skip_key='pe' if eng == 'pe' else None)
        self.ops[eng].append(['op', fn, waits, eng, idx])
        self._record(rb, wb, (eng, idx))

    def dma(self, qn, out, in_, extra_reads=(), **kw):
        rb, wb = self._boxes([in_] + list(extra_reads), [out])
        toks = self._deps(rb, wb)
        lanes = self.lanes[qn]
        li = self.lane_rr[qn]
        self.lane_rr[qn] = (li + 1) % len(lanes)
        lane = lanes[li]
        idx = self.cnt.get(lane, 0)
        self.cnt[lane] = idx + 1
        if idx > 0 and toks.get(lane, -1) < idx - 1:
            toks[lane] = idx - 1
        waits = self._waits_for(qn, toks)
        self.ops[qn].append(['dma', (out, in_, kw), waits, lane, idx])
        self._record(rb, wb, (lane, idx))

    def barrier(self, final=False):
        bg = (lambda kk: False) if final else (lambda kk: kk.startswith('d_pool'))
        toks = {k: c - 1 for k, c in self.cnt.items() if c > 0 and not bg(k)}
        for eng in self.ENG:
            waits = self._waits_for(eng, dict(toks))
            self.ops[eng].append(['wait', None, waits, None, None])
        keep = {}
        for nm, lst in self.wr.items():
            l2 = [(b, tk) for b, tk in lst if bg(tk[0])]
            if l2:
                keep[nm] = l2
        self.wr = keep
        self.rd = {}

    def emit(self):
        self.barrier(final=True)
        nc = self.nc
        rank = {}
        sems = {}
        for k, idxs in self.signal.items():
            isdma = k.startswith('d_')
            if isdma:
                n = self.cnt[k]
                rank[k] = None
                per = EPOCH // 16
                for ep in range((n + per - 1) // per):
                    sems[(k, ep)] = nc.alloc_semaphore(f'sm_{k}_{ep}')
            else:
                s = sorted(idxs)
                rank[k] = {ix: r for r, ix in enumerate(s)}
                for ep in range((len(s) + EPOCH - 1) // EPOCH):
                    sems[(k, ep)] = nc.alloc_semaphore(f'sm_{k}_{ep}')
        for k, n in self.cnt.items():
            if k.startswith('d_') and k not in rank:
                rank[k] = None
                per = EPOCH // 16
                for ep in range((n + per - 1) // per):
                    sems[(k, ep)] = nc.alloc_semaphore(f'sm_{k}_{ep}')
        self.n_sems = len(sems)

        def resolve(k, v):
            if rank[k] is None:
                per = EPOCH // 16
                return sems[(k, v // per)], ((v % per) + 1) * 16
            r = rank[k][v]
            return sems[(k, r // EPOCH)], (r % EPOCH) + 1

        def run_list(e, lst):
            for kind, payload, waits, mk, mi in lst:
                for k, v in waits:
                    s, val = resolve(k, v)
                    e.wait_ge(s, val)
                if kind == 'op':
                    ins = payload(e)
                    rk = rank.get(mk)
                    if rk is not None and mi in rk:
                        s, val = resolve(mk, mi)
                        ins.then_inc(s, 1)
                elif kind == 'dma':
                    out, in_, kw = payload
                    s, val = resolve(mk, mi)
                    e.dma_start(out=out, in_=in_, **kw).then_inc(s, 16)

        ops = self.ops
        with nc.Block() as block:
            @block.tensor
            def _(e):
                run_list(e, ops['pe'])

            @block.vector
            def _(e):
                run_list(e, ops['dve'])

            @block.scalar
            def _(e):
                run_list(e, ops['act'])

            @block.gpsimd
            def _(e):
                run_list(e, ops['pool'])

            @block.sync
            def _(e):
                run_list(e, ops['sp'])

    def mm(self, out, lhsT, rhs, start=True, stop=True):
        rd = [lhsT, rhs]
        self.op('pe', lambda e: e.matmul(out, lhsT, rhs, start=start, stop=stop), rd, [out])

    def tr(self, out, in_, ident):
        self.op('pe', lambda e: e.transpose(out, in_, ident), [in_, ident], [out])

    def act(self, out, in_, func, bias=None, scale=None, accum_out=None):
        kw = {}
        rd = [in_]
        wr = [out]
        if bias is not None:
            kw['bias'] = bias
            if not isinstance(bias, (int, float)):
                rd.append(bias)
        if scale is not None:
            kw['scale'] = scale
            if not isinstance(scale, (int, float)):
                rd.append(scale)
        if accum_out is not None:
            kw['accum_out'] = accum_out
            wr.append(accum_out)
        self.op('act', lambda e: e.activation(out, in_, func, **kw), rd, wr)

    def copy(self, eng, out, in_):
        if eng == 'act':
            self.op('act', lambda e: e.copy(out, in_), [in_], [out])
        else:
            self.op(eng, lambda e: e.tensor_copy(out, in_), [in_], [out])

    def tt(self, eng, out, in0, in1, op):
        self.op(eng, lambda e: e.tensor_tensor(out, in0, in1, op), [in0, in1], [out])

    def ts(self, eng, out, in0, s1, op0, s2=None, op1=None, accum_out=None):
        rd = [in0]
        if not isinstance(s1, (int, float)):
            rd.append(s1)
        if s2 is not None and not isinstance(s2, (int, float)):
            rd.append(s2)
        wr = [out]
        kw = {}
        if op1 is not None:
            kw['op1'] = op1
        if accum_out is not None:
            kw['accum_out'] = accum_out
            wr.append(accum_out)
        self.op(eng, lambda e: e.tensor_scalar(out, in0, s1, s2, op0, **kw), rd, wr)

    def stt(self, eng, out, in0, scalar, in1, op0, op1):
        rd = [in0, in1]
        if not isinstance(scalar, (int, float)):
            rd.append(scalar)
        self.op(eng, lambda e: e.scalar_tensor_tensor(out, in0, scalar, in1, op0, op1), rd, [out])

    def memset(self, eng, out, val):
        self.op(eng, lambda e: e.memset(out, val), [], [out])

D = 1024
T = 4096
CT = 256
NTOK = T + CT
NT = NTOK // 128
DEPTH = 2
NEXP = 32
ALPHA = (2 * DEPTH) ** 0.25
EPS = 1e-6
CA = (0, 512)
CBQ, CBK, CBV = 512, 768, 1024
CC0 = 1280
CD0 = 2560
SW_ALPHA = 1.702
SW_LIM = 7.0


def host_consts():
    c = {}
    c['ident'] = np.eye(128, dtype=np.float32)
    pos = np.arange(T)
    row = (pos // 64).astype(np.float32)
    col = (pos % 64).astype(np.float32)
    inv = (10000.0 ** (-np.arange(0, 16, 2, dtype=np.float32) / 16.0)).astype(np.float32)
    ar = row[:, None] * inv[None, :]
    ac = col[:, None] * inv[None, :]
    ang = np.concatenate([ar, ac], axis=1).astype(np.float32)
    c['rope'] = np.stack([np.cos(ang), np.sin(ang)], axis=1).astype(np.float32)
    s = np.arange(128)[:, None]
    t = np.arange(128)[None, :]
    same = (s // 32) == (t // 32)
    masks = np.stack([same & (s <= t), same & (s >= t), same & (s > t), same & (s < t)], 0)
    c['gmask'] = masks.astype(np.float32)
    ci = (np.arange(128)[:, None] // 32 == np.arange(4)[None, :]).astype(np.float32)
    c['cind'] = ci
    sel = np.zeros((2, 2, 128), np.float32)
    sel[0, 0, :] = 1.0
    sel[1, 1, :] = 1.0
    c['sel'] = sel
    return c


class K:
    pass


_UID = [0]


def UN(name):
    _UID[0] += 1
    return f"{name}_u{_UID[0]}"


def build_program(cfg):
    layers = cfg.get('layers', [0, 1])
    nexp = cfg.get('nexp', NEXP)
    phases = cfg.get('phases', 'ALL')
    dbg = cfg.get('dbg', [])
    nc = bass.Bass("TRN2", target_bir_lowering=False)
    S = Sched(nc)
    k = K()
    k.nc, k.S, k.cfg, k.nexp = nc, S, cfg, nexp
    k.last_layer = layers[-1]

    def din(name, shape, dt=F32):
        return nc.dram_tensor(name, list(shape), dt, kind="ExternalInput").ap()

    def dscr(name, shape, dt):
        return nc.dram_tensor(name, list(shape), dt, kind="Internal").ap()

    k.x = din('x', [T, D])
    k.ctx = din('ctx', [CT, D])
    k.cvec = din('cvec', [2, D])
    k.ada_w = din('ada_w', [DEPTH, D, 6 * D])
    k.ada_b = din('ada_b', [DEPTH, 6 * D])
    k.w_in = din('w_in', [DEPTH, D, 2976])
    k.w_out = din('w_out', [DEPTH, D, D])
    k.sgu_ln_w = din('sgu_ln_w', [DEPTH, 256])
    k.sgu_ln_b = din('sgu_ln_b', [DEPTH, 256])
    k.sgu_w = din('sgu_w', [DEPTH, 4, 128, 128])
    k.sgu_b = din('sgu_b', [DEPTH, 4, 128])
    k.diff_lambda = din('diff_lambda', [DEPTH, 4, 32])
    k.diff_subln_w = din('diff_subln_w', [DEPTH, 64])
    k.hlb = din('hgrn_lower_bounds', [DEPTH, 2, 256])
    k.hgrn_norm_w = din('hgrn_norm_w', [DEPTH, 64])
    k.mla_q_norm_w = din('mla_q_norm_w', [DEPTH, 256])
    k.mla_w_uq = din('mla_w_uq', [DEPTH, 256, 384])
    k.mla_kv_norm_w = din('mla_kv_norm_w', [DEPTH, 128])
    k.mla_w_ukv = din('mla_w_ukv', [DEPTH, 128, 512])
    k.ln_mix_w = din('ln_mix_w', [DEPTH, D])
    k.ln_mix_b = din('ln_mix_b', [DEPTH, D])
    k.ln_ffn_w = din('ln_ffn_w', [DEPTH, D])
    k.ln_ffn_b = din('ln_ffn_b', [DEPTH, D])
    k.router_w = din('router_w', [DEPTH, D, NEXP])
    k.router_b = din('router_b', [DEPTH, NEXP])
    NL = len(layers)
    k.w1 = din('expert_w1', [NL, nexp, D, 2 * D])
    k.b1 = din('expert_b1', [NL, nexp, 2 * D])
    k.w2 = din('expert_w2', [NL, nexp, D, D])
    k.b2 = din('expert_b2', [NL, nexp, D])
    k.c_ident = din('ident', [128, 128])
    k.c_rope = din('rope', [T, 2, 16])
    k.c_gmask = din('gmask', [4, 128, 128])
    k.c_cind = din('cind', [128, 4])
    k.c_sel = din('sel', [2, 2, 128])
    k.y = nc.dram_tensor('y', [T, D], F32, kind="ExternalOutput").ap()

    k.hT_d = dscr('hT_d', [NT, 128, 8, 128], BF16)
    k.cat_d = dscr('cat_d', [NTOK, D], BF16)
    k.xs_d = dscr('xs_d', [NTOK, D], F32)
    k.oF_d = dscr('oF_d', [NT, 64, 4, 128], F32)
    k.w1b_d = dscr('w1b_d', [NL, nexp, D, 2 * D], BF16)
    k.w2b_d = dscr('w2b_d', [NL, nexp, D, D], BF16)
    k.dbg_outs = {}

    def dbg_copy(name, src):
        if name not in dbg:
            return
        o = nc.dram_tensor('dbg_' + name, list(src.shape), src.dtype, kind="ExternalOutput").ap()
        k.dbg_outs[name] = o
        S.dma('sp', o, src)
    k.dbg_copy = dbg_copy

    with ExitStack() as es0:
        def sbp(name, shape, dt):
            return es0.enter_context(nc.sbuf_tensor(UN(name), list(shape), dt)).ap()
        k.ident_f = sbp('ident_f', [128, 128], F32)
        k.ident_b = sbp('ident_b', [128, 128], BF16)
        k.nhalf = sbp('nhalf', [128, 1], F32)
        S.dma('sp', k.ident_f, k.c_ident)
        S.copy('dve', k.ident_b, k.ident_f)
        k.epsb = sbp('epsb', [128, 1], F32)
        S.memset('dve', k.epsb, EPS)
        k.modT_l = {l: sbp(f'modT{l}', [128, 48, 2], F32) for l in layers}
        k.gates = sbp('gates', [128, NT, NEXP], F32)
        if phases == 'ALL' or '0' in phases:
            for l in layers:
                k.l = l
                k.modT = k.modT_l[l]
                phase_mod(k)
        k.conv_list = [(li_, e) for li_ in range(NL) for e in range(nexp)] if (phases == 'ALL' or 'M' in phases) else []

        def conv_step(gate=None, n=1):
            for _ in range(n):
                if not k.conv_list:
                    return
                li_, e = k.conv_list.pop(0)
                er = [gate] if gate is not None else []
                for h in range(2):
                    S.dma('pool', k.w1b_d[li_, e, h * 512:(h + 1) * 512, :], k.w1[li_, e, h * 512:(h + 1) * 512, :], extra_reads=er)
                S.dma('pool', k.w2b_d[li_, e], k.w2[li_, e], extra_reads=er)
        k.conv_step = conv_step
        for l in layers:
            k.l = l
            k.modT = k.modT_l[l]
            k.need_ctx = (l < DEPTH - 1)
            k.t0 = 0 if k.need_ctx else 2
            if phases == 'ALL' or '1' in phases:
                phase_ln1(k)
            if phases == 'ALL' or 'A' in phases:
                phase_A(k)
            if phases == 'ALL' or 'B' in phases:
                phase_B(k)
            if phases == 'ALL' or 'C' in phases:
                phase_C(k)
            if phases == 'ALL' or 'D' in phases:
                phase_D(k)
            if l == layers[0]:
                dbg_copy('cat', k.cat_d)
            if phases == 'ALL' or 'O' in phases:
                phase_O(k)
            if l == layers[0]:
                dbg_copy('x1', k.xs_d)
                dbg_copy('h2T', k.hT_d)
            if phases == 'ALL' or 'M' in phases:
                phase_M(k)
            if l == layers[0]:
                dbg_copy('xout', k.xs_d)
        S.emit()
    return nc, k


def x_src(k, i):
    if k.l == 0:
        if i < 2:
            return k.ctx[i * 128:(i + 1) * 128, :]
        return k.x[(i - 2) * 128:(i - 1) * 128, :]
    return k.xs_d[i * 128:(i + 1) * 128, :]


def mcol(k, i):
    return 1 if i < 2 else 0


class Ring:
    def __init__(self, es, nc, name, shape, dt, n, psum=False):
        self.b = []
        for j in range(n):
            if psum:
                self.b.append(es.enter_context(nc.psum_tensor(UN(f'{name}{j}'), list(shape), dt)).ap())
            else:
                self.b.append(es.enter_context(nc.sbuf_tensor(UN(f'{name}{j}'), list(shape), dt)).ap())
        self.i = 0

    def next(self):
        r = self.b[self.i % len(self.b)]
        self.i += 1
        return r


def rsqrt_act(k, dst, src, tmp, scale, eps):
    k.S.act(tmp, src, AF.Ln, bias=k.epsb if eps == EPS else eps, scale=scale)
    k.S.act(dst, tmp, AF.Exp, scale=-0.5)


def ln_stats(k, src, n, st, eps=EPS):
    S = k.S
    nch = max(1, n // 512)
    w = n // nch
    for c in range(nch):
        S.op('dve', lambda e, c=c: e.bn_stats(st['s6'][:, c, :], src[:, c * w:(c + 1) * w]),
             [src[:, c * w:(c + 1) * w]], [st['s6'][:, c, :]])
    S.op('dve', lambda e: e.bn_aggr(st['mv'], st['s6'][:, 0:nch, :]), [st['s6'][:, 0:nch, :]], [st['mv']])
    rsqrt_act(k, st['rstd'], st['mv'][:, 1:2], st['ve'], 1.0, eps)
    S.stt('dve', st['nmr'], st['mv'][:, 0:1], -1.0, st['rstd'], ALU.mult, ALU.mult)


def stat_tiles(es, nc, name, n=2):
    out = []
    for j in range(n):
        d = {}
        for nm, shp in (('s6', [128, 2, 6]), ('mv', [128, 2]), ('ve', [128, 1]), ('rstd', [128, 1]), ('nmr', [128, 1])):
            d[nm] = es.enter_context(nc.sbuf_tensor(UN(f'{name}_{nm}{j}'), shp, F32)).ap()
        out.append(d)
    return out


def phase_mod(k):
    nc, S, l = k.nc, k.S, k.l
    with ExitStack() as es:
        sb = lambda n, s, d: es.enter_context(nc.sbuf_tensor(UN(n), list(s), d)).ap()
        cv = sb('m_cv', [128, 8, 2], F32)
        scv = sb('m_scv', [128, 8, 2], F32)
        sig = sb('m_sig', [128, 8, 2], F32)
        adab = sb('m_adab', [2, 6 * D], F32)
        modrow = sb('m_modrow', [2, 6 * D], F32)
        sel = sb('m_sel', [2, 2, 128], F32)
        aw = Ring(es, nc, 'm_aw', [128, 8, 512], F32, 2)
        pm = Ring(es, nc, 'm_pm', [128, 512], F32, 2, psum=True)
        pt = es.enter_context(nc.psum_tensor(UN('m_pt'), [128, 512], F32)).ap()
        for r in range(2):
            S.dma('sp', cv[:, :, r], k.cvec[r, :].rearrange("(kk p) -> p kk", p=128), allow_slow_non_contiguous=True)
        S.dma('sp', adab, k.ada_b[l:l + 1, :].to_broadcast([2, 6 * D]))
        S.dma('sp', sel, k.c_sel)
        S.act(sig, cv, AF.Sigmoid)
        S.tt('dve', scv, cv, sig, ALU.mult)
        for nb in range(12):
            a = aw.next()
            S.dma('sp', a, k.ada_w[l, :, nb * 512:(nb + 1) * 512].rearrange("(kk p) n -> p kk n", p=128))
            p = pm.next()
            for kk in range(8):
                S.mm(p[0:2, :], scv[:, kk, :], a[:, kk, :], start=(kk == 0), stop=(kk == 7))
            S.tt('dve', modrow[:, nb * 512:(nb + 1) * 512], p[0:2, :], adab[:, nb * 512:(nb + 1) * 512], ALU.add)
        ptv = pt[:, 0:96].rearrange("p (j r) -> p j r", r=2)
        for j in range(48):
            S.tr(ptv[:, j, :], modrow[0:2, j * 128:(j + 1) * 128], k.ident_f[0:2, 0:2])
        S.copy('dve', k.modT, ptv)
        for c0 in (8, 32):
            S.ts('dve', k.modT[:, c0:c0 + 8, :], k.modT[:, c0:c0 + 8, :], 1.0, ALU.add)
        if not hasattr(k, 'gate_d'):
            k.gate_d = nc.dram_tensor('gate_d', [DEPTH, 2, 2, D], F32, kind="Internal").ap()
        S.dma('sp', k.gate_d[l, :, 0, :], modrow[:, 2 * D:3 * D])
        S.dma('sp', k.gate_d[l, :, 1, :], modrow[:, 5 * D:6 * D])
    S.barrier()


def emit_hT(k, xn, i, sc0, sh0, pT_ring, hT_ring, dt_is_f32=False):
    S = k.S
    r = mcol(k, i)
    pT = pT_ring.next()
    pTv = pT.rearrange("p (kk t) -> p kk t", kk=8)
    for kk in range(8):
        S.tr(pTv[:, kk, :], xn[:, kk * 128:(kk + 1) * 128], k.ident_b)
    hT = hT_ring.next()
    for kk in range(8):
        sc = k.modT[:, sc0 + kk, r:r + 1]
        sh = k.modT[:, sh0 + kk, r:r + 1]
        if kk % 2 == 0:
            S.act(hT[:, kk, :], pTv[:, kk, :], AF.Identity, bias=sh, scale=sc)
        else:
            S.ts('dve', hT[:, kk, :], pTv[:, kk, :], sc, ALU.mult, sh, ALU.add)
    S.dma('sp', k.hT_d[i], hT)
    return hT


def phase_ln1(k, after=None):
    nc, S = k.nc, k.S
    with ExitStack() as es:
        xt = Ring(es, nc, 'l1_x', [128, D], F32, 2)
        xn = Ring(es, nc, 'l1_xn', [128, D], BF16, 2)
        hT = Ring(es, nc, 'l1_hT', [128, 8, 128], BF16, 2)
        pT = Ring(es, nc, 'l1_pT', [128, 1024], BF16, 2, psum=True)
        sts = stat_tiles(es, nc, 'l1')
        def stage_a(i):
            x = xt.next()
            S.dma('sp', x, x_src(k, i))
            st = sts[i % 2]
            ln_stats(k, x, D, st)
            n = xn.next()
            S.act(n, x, AF.Identity, bias=st['nmr'], scale=st['rstd'])
            return n
        nxt = stage_a(0)
        for i in range(NT):
            cur = nxt
            if i + 1 < NT:
                nxt = stage_a(i + 1)
            emit_hT(k, cur, i, 8, 0, pT, hT)
        if after is not None:
            after()
    S.barrier()
    if k.l == k.cfg.get('layers', [0, 1])[0]:
        k.dbg_copy('hT', k.hT_d)


def load_w(k, dst, src_cols, stage):
    c0, c1 = src_cols
    n = c1 - c0
    j = 0
    for a in range(0, n, 256):
        b = min(n, a + 256)
        st = stage.next()
        k.S.dma('sp', st[:, :, 0:b - a], k.w_in[k.l, :, c0 + a:c0 + b].rearrange("(kk p) n -> p kk n", p=128))
        k.S.copy('act' if j % 2 else 'dve', dst[:, :, a:b], st[:, :, 0:b - a])
        j += 1


def wstage(es, nc, name):
    return Ring(es, nc, name, [128, 8, 256], F32, 2)


def proj(k, out_ps, hT, w, n0, n1):
    for kk in range(8):
        k.S.mm(out_ps, hT[:, kk, :], w[:, kk, n0:n1], start=(kk == 0), stop=(kk == 7))


def phase_A(k):
    nc, S, l = k.nc, k.S, k.l
    with ExitStack() as es:
        sb = lambda n, s, d: es.enter_context(nc.sbuf_tensor(UN(n), list(s), d)).ap()
        wA = sb('a_w', [128, 8, 512], BF16)
        stg = wstage(es, nc, 'a_stg')
        load_w(k, wA, (0, 512), stg)
        ws = sb('a_ws', [128, 4, 128], F32)
        wsT = sb('a_wsT', [128, 4, 128], BF16)
        bs4 = sb('a_bs4', [128, 4], F32)
        BS = sb('a_BS', [128, 4, 64], F32)
        LW = sb('a_LW', [128, 256], F32)
        LB = sb('a_LB', [128, 256], F32)
        S.dma('sp', ws, k.sgu_w[l].rearrange("g t s -> t g s"))
        S.dma('sp', bs4, k.sgu_b[l].rearrange("g t -> t g"), allow_slow_non_contiguous=True)
        S.dma('sp', LW, k.sgu_ln_w[l:l + 1, :].to_broadcast([128, 256]))
        S.dma('sp', LB, k.sgu_ln_b[l:l + 1, :].to_broadcast([128, 256]))
        pw = es.enter_context(nc.psum_tensor(UN('a_pw'), [128, 512], F32)).ap()
        pwv = pw.rearrange("p (g t) -> p g t", g=4)
        for g in range(4):
            S.tr(pwv[:, g, :], ws[:, g, :], k.ident_f)
        S.copy('dve', wsT, pwv)
        S.copy('dve', BS, bs4.unsqueeze(2).to_broadcast([128, 4, 64]))
        hTr = Ring(es, nc, 'a_hT', [128, 8, 128], BF16, 2)
        pA = Ring(es, nc, 'a_pA', [128, 512], F32, 2, psum=True)
        pM = Ring(es, nc, 'a_pM', [128, 512], F32, 2, psum=True)
        uvg = Ring(es, nc, 'a_uvg', [128, 512], F32, 2)
        vn = Ring(es, nc, 'a_vn', [128, 256], F32, 2)
        vn2 = Ring(es, nc, 'a_vn2', [128, 256], F32, 2)
        vnb = Ring(es, nc, 'a_vnb', [128, 256], BF16, 2)
        t1 = Ring(es, nc, 'a_t1', [128, 256], F32, 2)
        oa = Ring(es, nc, 'a_oa', [128, 256], BF16, 2)
        sts = stat_tiles(es, nc, 'a')
        def stage_a(i):
            hT = hTr.next()
            S.dma('sp', hT, k.hT_d[i])
            p = pA.next()
            proj(k, p, hT, wA, 0, 512)
            u = uvg.next()
            S.act(u, p, AF.Gelu)
            return u

        def stage_b(i, u):
            st = sts[i % 2]
            ln_stats(k, u[:, 256:512], 256, st)
            v = vn.next()
            S.act(v, u[:, 256:512], AF.Identity, bias=st['nmr'], scale=st['rstd'])
            v2 = vn2.next()
            S.tt('dve', v2, v, LW, ALU.mult)
            vb = vnb.next()
            S.tt('dve', vb, v2, LB, ALU.add)
            pm = pM.next()
            for g in range(4):
                S.mm(pm[:, g * 64:(g + 1) * 64], wsT[:, g, :], vb[:, g * 64:(g + 1) * 64], start=True, stop=True)
            tt1 = t1.next()
            S.tt('dve', tt1, pm[:, 0:256], BS.rearrange("p g c -> p (g c)"), ALU.add)
            o = oa.next()
            S.tt('dve', o, tt1, u[:, 0:256], ALU.mult)
            S.dma('sp', k.cat_d[i * 128:(i + 1) * 128, 0:256], o)
        tl = list(range(k.t0, NT))
        nxt = stage_a(tl[0])
        for n, i in enumerate(tl):
            cur = nxt
            if n + 1 < len(tl):
                nxt = stage_a(tl[n + 1])
            stage_b(i, cur)
    S.barrier()


def rope_apply(k, src5, dst5, cs, G, tmp):
    S = k.S
    cos = cs[:, 0, :].rearrange("p (a f) -> p a f", a=2).unsqueeze(1).to_broadcast([128, G, 2, 8])
    sin = cs[:, 1, :].rearrange("p (a f) -> p a f", a=2).unsqueeze(1).to_broadcast([128, G, 2, 8])
    x1 = src5[:, :, :, 0, :]
    x2 = src5[:, :, :, 1, :]
    ta, tb, tc, td = [t[:, 0:G, :, :] for t in tmp]
    S.tt('dve', ta, x1, cos, ALU.mult)
    S.tt('dve', tb, x2, sin, ALU.mult)
    S.tt('dve', dst5[:, :, :, 0, :], ta, tb, ALU.subtract)
    S.tt('dve', tc, x2, cos, ALU.mult)
    S.tt('dve', td, x1, sin, ALU.mult)
    S.tt('dve', dst5[:, :, :, 1, :], tc, td, ALU.add)


def attention_res(k, es, npairs):
    nc = k.nc
    pS = Ring(es, nc, 'at_pS', [128, 512], F32, 4, psum=True)
    pO = Ring(es, nc, 'at_pO', [128, 512], F32, 2, psum=True)
    pexp = Ring(es, nc, 'at_pe', [128, 512], BF16, 4)
    oTs = Ring(es, nc, 'at_oT', [65, npairs, 512], F32, 2)
    return (pS, pO, pexp, oTs)


def attention_core(k, es, qT, kT, v1, nk_part, scale, pairs, finalize, LA=3):
    nc, S = k.nc, k.S
    pS, pO, pexp, oTs = es
    blocks = []
    if k.need_ctx:
        blocks.append((0, 256, [0, 1]))
    for j in range(8):
        blocks.append((256 + j * 512, 512, list(range(NT))))
    steps = []
    for (q0, qn, kts) in blocks:
        for pi in range(len(pairs)):
            for n, kt in enumerate(kts):
                steps.append((q0, qn, pi, n, kt, len(kts)))

    def issue_score(st):
        q0, qn, pi, n, kt, nk = st
        g, pb, hh = pairs[pi]
        ps = pS.next()
        if callable(g):
            ka, qa = g(kt, q0, qn)
            S.mm(ps[:, 0:qn], ka, qa)
        else:
            S.mm(ps[:, 0:qn], kT[pb:pb + nk_part, g, kt * 128:(kt + 1) * 128], qT[pb:pb + nk_part, g, q0:q0 + qn])
        return ps
    inflight = [issue_score(st) for st in steps[0:LA]]
    po = None
    o_sb = None
    for si, st in enumerate(steps):
        q0, qn, pi, n, kt, nk = st
        g, pb, hh = pairs[pi]
        if si + LA < len(steps):
            inflight.append(issue_score(steps[si + LA]))
        ps = inflight.pop(0)
        if n == 0:
            po = pO.next()
            if pi == 0:
                o_sb = oTs.next()
        pe = pexp.next()
        S.act(pe[:, 0:qn], ps[:, 0:qn], AF.Exp, scale=scale)
        S.mm(po[0:65, 0:qn], v1[:, kt, hh, :], pe[:, 0:qn], start=(n == 0), stop=(n == nk - 1))
        if n == nk - 1:
            S.copy('dve', o_sb[:, pi, 0:qn], po[0:65, 0:qn])
            if pi == len(pairs) - 1:
                for j in range(qn // 128):
                    finalize((q0 // 128) + j, o_sb, j)


def phase_B(k):
    nc, S, l = k.nc, k.S, k.l
    lam_init = 0.8 - 0.6 * math.exp(-0.3 * l)
    scale = 32 ** -0.5
    for hp in range(2):
        with ExitStack() as es:
            sb = lambda n, s, d: es.enter_context(nc.sbuf_tensor(UN(n), list(s), d)).ap()
            wB = sb('b_w', [128, 8, 384], BF16)
            stg = wstage(es, nc, 'b_stg')
            for j, c0 in enumerate((CBQ, CBK)):
                st = stg.next()
                S.dma('sp', st[:, :, 0:128],
                      k.w_in[l, :, c0 + hp * 128:c0 + hp * 128 + 128].rearrange("(kk p) n -> p kk n", p=128))
                sv = st[:, :, 0:128].rearrange("p kk (hh m d) -> p kk hh m d", hh=2, m=2)
                for m in range(2):
                    S.copy('dve' if m else 'act',
                           wB[:, :, j * 128 + m * 64:j * 128 + (m + 1) * 64].rearrange("p kk (hh d) -> p kk hh d", hh=2),
                           sv[:, :, :, m, :])
            load_w(k, wB[:, :, 256:384], (CBV + hp * 128, CBV + hp * 128 + 128), stg)
            qT = sb('b_qT', [128, NTOK], BF16)
            kT = sb('b_kTz', [128, 4, NTOK], BF16)
            rm = sb('b_rm', [128, 4], F32)
            S.dma('sp', rm, k.c_cind)
            v1 = sb('b_v1', [128, NT, 2, 65], BF16)
            oball = sb('b_ob', [128, NT, 2, 64], BF16)
            S.memset('dve', v1[:, :, :, 64:65], 1.0)
            lamt = sb('b_lam', [128, 4, 32], F32)
            lp = sb('b_lp', [128, 2, 32], F32)
            ls = sb('b_ls', [128, 2], F32)
            le = sb('b_le', [128, 2], F32)
            nlam = sb('b_nlam', [128, 1], F32)
            SUBW = sb('b_subw', [128, 64], F32)
            S.dma('sp', lamt, k.diff_lambda[l:l + 1].to_broadcast([128, 4, 32]))
            S.dma('sp', SUBW, k.diff_subln_w[l:l + 1, :].to_broadcast([128, 64]))
            S.ts('dve', SUBW, SUBW, 1.0 - lam_init, ALU.mult)
            lv = lamt.rearrange("p (a b) d -> p a b d", b=2)
            S.tt('dve', lp, lv[:, :, 0, :], lv[:, :, 1, :], ALU.mult)
            S.op('dve', lambda e: e.tensor_reduce(ls, lp, AX.X, ALU.add), [lp], [ls])
            S.act(le, ls, AF.Exp)
            S.tt('dve', nlam, le[:, 1:2], le[:, 0:1], ALU.subtract)
            S.ts('dve', nlam, nlam, -lam_init, ALU.add)
            with ExitStack() as es1:
                hTr = Ring(es1, nc, 'b_hT', [128, 8, 128], BF16, 2)
                pB = Ring(es1, nc, 'b_pB', [128, 512], F32, 2, psum=True)
                pT = Ring(es1, nc, 'b_pT', [128, 1024], BF16, 2, psum=True)
                csr = Ring(es1, nc, 'b_cs', [128, 2, 16], F32, 2)
                qkr = Ring(es1, nc, 'b_qkr', [128, 256], BF16, 2)
                tmp = [es1.enter_context(nc.sbuf_tensor(UN(f'b_tmp{j}'), [128, 8, 2, 8], F32)).ap() for j in range(4)]
                for i in range(NT):
                    hT = hTr.next()
                    S.dma('sp', hT, k.hT_d[i])
                    p = pB.next()
                    proj(k, p[:, 0:384], hT, wB, 0, 384)
                    qk = qkr.next()
                    if i >= 2:
                        cs = csr.next()
                        S.dma('sp', cs, k.c_rope[(i - 2) * 128:(i - 1) * 128])
                        src5 = p[:, 0:256].rearrange("p (g a h f) -> p g a h f", g=8, a=2, h=2)
                        dst5 = qk.rearrange("p (g a h f) -> p g a h f", g=8, a=2, h=2)
                        rope_apply(k, src5, dst5, cs, 8, tmp)
                    else:
                        S.copy('act', qk, p[:, 0:256])
                    S.copy('act', v1[:, i, :, 0:64], p[:, 256:384].rearrange("p (h c) -> p h c", h=2))
                    pt = pT.next()
                    ptv = pt[:, 0:256].rearrange("p (w t) -> p w t", w=2)
                    for w in range(2):
                        S.tr(ptv[:, w, :], qk[:, w * 128:(w + 1) * 128], k.ident_b)
                    S.copy('dve', qT[:, i * 128:(i + 1) * 128], ptv[:, 0, :])
                    for j in range(4):
                        if j % 2 == 0:
                            S.act(kT[:, j, i * 128:(i + 1) * 128], ptv[:, 1, :], AF.Identity, scale=rm[:, j:j + 1])
                        else:
                            S.ts('dve', kT[:, j, i * 128:(i + 1) * 128], ptv[:, 1, :], rm[:, j:j + 1], ALU.mult)
            S.barrier()
            with ExitStack() as es2:
                sb2 = lambda n, s, d: es2.enter_context(nc.sbuf_tensor(UN(n), list(s), d)).ap()
                pF = Ring(es2, nc, 'b_pF', [128, 512], F32, 2, psum=True)
                rden = Ring(es2, nc, 'b_rden', [128, 2], F32, 2)
                a0r = Ring(es2, nc, 'b_a0', [128, 64], F32, 2)
                ar = Ring(es2, nc, 'b_a', [128, 64], F32, 2)
                sqr = Ring(es2, nc, 'b_sq', [128, 64], F32, 2)
                ssr = Ring(es2, nc, 'b_ss', [128, 1], F32, 2)
                rsr = Ring(es2, nc, 'b_rs', [128, 1], F32, 2)
                nl1 = Ring(es2, nc, 'b_nl1', [128, 1], F32, 2)
                ares = attention_res(k, es2, 2)
                for hh in range(2):
                    def fin(ti, o_sb, j, hh=hh):
                        pf = pF.next()
                        pfv = pf[:, 0:256].rearrange("p (m c) -> p m c", m=2)
                        for m in range(2):
                            S.tr(pfv[:, m, 0:65], o_sb[0:65, m, j * 128:(j + 1) * 128], k.ident_f[0:65, 0:65])
                        rd = rden.next()
                        S.op('dve', lambda e: e.reciprocal(rd, pfv[:, :, 64]), [pfv[:, :, 64]], [rd])
                        a0 = a0r.next()
                        S.ts('dve', a0, pfv[:, 0, 0:64], rd[:, 0:1], ALU.mult)
                        n1 = nl1.next()
                        S.tt('dve', n1, rd[:, 1:2], nlam, ALU.mult)
                        a = ar.next()
                        S.stt('dve', a, pfv[:, 1, 0:64], n1, a0, ALU.mult, ALU.add)
                        sq = sqr.next()
                        ss = ssr.next()
                        S.act(sq, a, AF.Square, accum_out=ss)
                        rs = rsr.next()
                        rsqrt_act(k, rs, ss, ss, 1.0 / 64.0, EPS)
                        S.stt('dve', oball[:, ti, hh, :], a, rs, SUBW, ALU.mult, ALU.mult)
                        if ti >= 2 and (ti - 2) % 4 == 3:
                            k.conv_step(oball[0:1, ti, hh, 0:1])
                    def getter(m, hh=hh):
                        return lambda kt, q0, qn: (kT[:, m * 2 + hh, kt * 128:(kt + 1) * 128], qT[:, q0:q0 + qn])
                    attention_core(k, ares, qT, kT, v1, 128, scale, [(getter(0), 0, hh), (getter(1), 0, hh)], fin)
                for i in range(k.t0, NT):
                    S.dma('sp', k.cat_d[i * 128:(i + 1) * 128, 256 + hp * 128:256 + (hp + 1) * 128],
                          oball[:, i, :, :].rearrange("p h c -> p (h c)"))
        S.barrier()


def phase_D(k):
    nc, S, l = k.nc, k.S, k.l
    scale = 96 ** -0.5
    for hp in range(2):
        with ExitStack() as es:
            sb = lambda n, s, d: es.enter_context(nc.sbuf_tensor(UN(n), list(s), d)).ap()
            stg = wstage(es, nc, 'd_stg')
            wD = sb('d_w', [128, 8, 416], BF16)
            load_w(k, wD, (CD0, CD0 + 416), stg)
            wuq_f = sb('d_wuqf', [128, 2, 192], F32)
            S.dma('sp', wuq_f, k.mla_w_uq[l, :, hp * 192:(hp + 1) * 192].rearrange("(c p) n -> p c n", p=128))
            qnw = sb('d_qnw', [128, 2], F32)
            S.dma('sp', qnw, k.mla_q_norm_w[l].rearrange("(c p) -> p c", p=128), allow_slow_non_contiguous=True)
            wuq = sb('d_wuq', [128, 2, 192], BF16)
            for c in range(2):
                S.ts('dve', wuq[:, c, :], wuq_f[:, c, :], qnw[:, c:c + 1], ALU.mult)
            wukv_f = sb('d_wukvf', [128, 256], F32)
            S.dma('sp', wukv_f, k.mla_w_ukv[l, :, hp * 256:(hp + 1) * 256])
            kvw = sb('d_kvw', [128, 1], F32)
            S.dma('sp', kvw, k.mla_kv_norm_w[l].rearrange("(p o) -> p o", o=1))
            wukv = sb('d_wukv', [128, 256], BF16)
            S.ts('dve', wukv, wukv_f, kvw, ALU.mult)
            qT = sb('d_qT', [96, 2, NTOK], BF16)
            kT = sb('d_kT', [96, 2, NTOK], BF16)
            v1 = sb('d_v1', [128, NT, 2, 65], BF16)
            oball = sb('d_ob', [128, NT, 2, 64], BF16)
            S.memset('dve', v1[:, :, :, 64:65], 1.0)
            with ExitStack() as es1:
                sb1 = lambda n, s, d: es1.enter_context(nc.sbuf_tensor(UN(n), list(s), d)).ap()
                hTr = Ring(es1, nc, 'd_hT', [128, 8, 128], BF16, 2)
                pD = Ring(es1, nc, 'd_pD', [128, 512], F32, 2, psum=True)
                pQ = Ring(es1, nc, 'd_pQ', [128, 512], F32, 2, psum=True)
                pT = Ring(es1, nc, 'd_pT', [128, 1024], BF16, 2, psum=True)
                csr = Ring(es1, nc, 'd_cs', [128, 2, 16], F32, 2)
                junk = Ring(es1, nc, 'd_junk', [128, 256], F32, 2)
                ssr = Ring(es1, nc, 'd_ss', [128, 2], F32, 2)
                lnr = Ring(es1, nc, 'd_ln', [128, 2], F32, 2)
                rrr = Ring(es1, nc, 'd_rr', [128, 2], F32, 2)
                cbr = Ring(es1, nc, 'd_cb', [128, 384], BF16, 2)
                cTr = Ring(es1, nc, 'd_cT', [128, 3, 128], BF16, 2)
                qfr = Ring(es1, nc, 'd_qf', [128, 2, 96], BF16, 2)
                kfr = Ring(es1, nc, 'd_kf', [128, 2, 96], BF16, 2)
                qrr = Ring(es1, nc, 'd_qr', [128, 2, 32], F32, 2)
                tmp = [sb1(f'd_tmp{j}', [128, 2, 2, 8], F32) for j in range(4)]
                for i in range(NT):
                    hT = hTr.next()
                    S.dma('sp', hT, k.hT_d[i])
                    p = pD.next()
                    proj(k, p[:, 0:416], hT, wD, 0, 416)
                    jk = junk.next()
                    ss = ssr.next()
                    S.act(jk[:, 0:256], p[:, 0:256], AF.Square, accum_out=ss[:, 0:1])
                    S.act(jk[:, 0:128], p[:, 256:384], AF.Square, accum_out=ss[:, 1:2])
                    ln = lnr.next()
                    rr = rrr.next()
                    rsqrt_act(k, rr[:, 0:1], ss[:, 0:1], ln[:, 0:1], 1.0 / 256.0, EPS)
                    rsqrt_act(k, rr[:, 1:2], ss[:, 1:2], ln[:, 1:2], 1.0 / 128.0, EPS)
                    cb = cbr.next()
                    S.copy('act', cb, p[:, 0:384])
                    pt = pT.next()
                    ptv = pt[:, 0:384].rearrange("p (c t) -> p c t", c=3)
                    for c in range(3):
                        S.tr(ptv[:, c, :], cb[:, c * 128:(c + 1) * 128], k.ident_b)
                    cT = cTr.next()
                    S.copy('dve', cT, ptv)
                    pq = pQ.next()
                    S.mm(pq[:, 0:192], cT[:, 0, :], wuq[:, 0, :], start=True, stop=False)
                    S.mm(pq[:, 0:192], cT[:, 1, :], wuq[:, 1, :], start=False, stop=True)
                    S.mm(pq[:, 256:512], cT[:, 2, :], wukv, start=True, stop=True)
                    pqv = pq[:, 0:192].rearrange("p (h c) -> p h c", h=2)
                    pkv = pq[:, 256:512].rearrange("p (h c) -> p h c", h=2)
                    qf = qfr.next()
                    kf = kfr.next()
                    rq = rr[:, 0:1]
                    rkv = rr[:, 1:2]
                    S.ts('dve', qf[:, :, 0:64], pqv[:, :, 0:64], rq, ALU.mult)
                    S.ts('dve', kf[:, :, 0:64], pkv[:, :, 0:64], rkv, ALU.mult)
                    S.act(v1[:, i, :, 0:64], pkv[:, :, 64:128], AF.Identity, scale=rkv)
                    if i >= 2:
                        cs = csr.next()
                        S.dma('sp', cs, k.c_rope[(i - 2) * 128:(i - 1) * 128])
                        qr = qrr.next()
                        S.ts('dve', qr, pqv[:, :, 64:96], rq, ALU.mult)
                        rope_apply(k, qr.rearrange("p g (a h f) -> p g a h f", a=2, h=2),
                                   qf[:, :, 64:96].rearrange("p g (a h f) -> p g a h f", a=2, h=2), cs, 2, tmp)
                        rope_apply(k, p[:, 384:416].rearrange("p (g a h f) -> p g a h f", g=1, a=2, h=2),
                                   kf[:, 0:1, 64:96].rearrange("p g (a h f) -> p g a h f", a=2, h=2), cs, 1, tmp)
                        S.copy('dve', kf[:, 1, 64:96], kf[:, 0, 64:96])
                    else:
                        S.ts('dve', qf[:, :, 64:96], pqv[:, :, 64:96], rq, ALU.mult)
                        S.copy('dve', kf[:, :, 64:96], p[:, 384:416].unsqueeze(1).to_broadcast([128, 2, 32]))
                    pt2 = pT.next()
                    pt2v = pt2[0:96, 0:512].rearrange("p (w t) -> p w t", w=4)
                    for hh in range(2):
                        S.tr(pt2v[:, hh, :], qf[:, hh, :], k.ident_b)
                        S.tr(pt2v[:, 2 + hh, :], kf[:, hh, :], k.ident_b)
                    S.copy('act', qT[:, :, i * 128:(i + 1) * 128], pt2v[:, 0:2, :])
                    S.copy('dve', kT[:, :, i * 128:(i + 1) * 128], pt2v[:, 2:4, :])
            S.barrier()
            with ExitStack() as es2:
                pF = Ring(es2, nc, 'd_pF', [128, 512], F32, 2, psum=True)
                rden = Ring(es2, nc, 'd_rden', [128, 1], F32, 2)
                ares = attention_res(k, es2, 1)
                for hh in range(2):
                    def fin(ti, o_sb, j, hh=hh):
                        pf = pF.next()
                        S.tr(pf[:, 0:65], o_sb[0:65, 0, j * 128:(j + 1) * 128], k.ident_f[0:65, 0:65])
                        rd = rden.next()
                        S.op('dve', lambda e: e.reciprocal(rd, pf[:, 64:65]), [pf[:, 64:65]], [rd])
                        S.ts('dve', oball[:, ti, hh, :], pf[:, 0:64], rd, ALU.mult)
                        if ti >= 2 and (ti - 2) % 4 == 3:
                            k.conv_step(oball[0:1, ti, hh, 0:1])
                    attention_core(k, ares, qT, kT, v1, 96, scale, [(hh, 0, hh)], fin)
                for i in range(k.t0, NT):
                    S.dma('sp', k.cat_d[i * 128:(i + 1) * 128, 768 + hp * 128:768 + (hp + 1) * 128],
                          oball[:, i, :, :].rearrange("p h c -> p (h c)"))
        S.barrier()


def phase_C(k):
    nc, S, l = k.nc, k.S, k.l
    with ExitStack() as es:
        sb = lambda n, s, d: es.enter_context(nc.sbuf_tensor(UN(n), list(s), d)).ap()
        stg = wstage(es, nc, 'c_stg')
        wC = sb('c_w', [128, 8, 1280], BF16)
        load_w(k, wC, (CC0, CC0 + 1280), stg)
        gm = sb('c_gm', [128, 4, 128], F32)
        S.dma('sp', gm, k.c_gmask.rearrange("m s t -> s m t"))
        ci = sb('c_ci', [128, 4], F32)
        S.dma('sp', ci, k.c_cind)
        NW = sb('c_NW', [128, 64], F32)
        S.dma('sp', NW, k.hgrn_norm_w[l:l + 1, :].to_broadcast([128, 64]))
        if l > 0:
            LBt = sb('c_LB', [128, 2, 256], F32)
            OML = sb('c_OML', [128, 2, 256], F32)
            h0 = sb('c_h0', [128, 2, 256], F32)
            S.dma('sp', h0, k.hlb[0:1].to_broadcast([128, 2, 256]))
            S.dma('sp', LBt, k.hlb[1:2].to_broadcast([128, 2, 256]))
            S.tt('dve', h0, LBt, h0, ALU.subtract)
            S.act(LBt, h0, AF.Sigmoid)
            S.ts('dve', OML, LBt, -1.0, ALU.mult, 1.0, ALU.add)
        S_st = sb('c_S', [64, 4, 64], F32)
        tmpS = sb('c_tmpS', [64, 4, 64], F32)
        ps = lambda n, dt=F32, w=512: es.enter_context(nc.psum_tensor(UN(n), [128, w], dt)).ap()
        p1, p2, pb, pSc, pdS, poT, pm = [ps(n) for n in ('c_p1', 'c_p2', 'c_pb', 'c_pS', 'c_pdS', 'c_poT', 'c_pm')]
        pT = ps('c_pT', BF16, 1024)
        hTr = Ring(es, nc, 'c_hT', [128, 8, 128], BF16, 2)
        f32t = lambda nm: Ring(es, nc, nm, [128, 256], F32, 2)
        qhr, sgr, fr, kkr, lfr, e1r, e2r, e3r, krfr = [f32t(n) for n in
                                                       ('c_qh', 'c_sg', 'c_f', 'c_kk', 'c_lf', 'c_e1', 'c_e2', 'c_e3', 'c_krf')]
        qdr = Ring(es, nc, 'c_qd', [128, 256], BF16, 2)
        kdr = Ring(es, nc, 'c_kd', [128, 256], BF16, 2)
        kr4r = Ring(es, nc, 'c_kr4', [128, 4, 256], BF16, 2)
        slots = []
        for j in range(2):
            d_ = {}
            d_['qkT'] = sb(f'c_qkT{j}', [64, 8, 128], BF16)
            d_['A'] = sb(f'c_A{j}', [128, 4, 128], BF16)
            d_['dS'] = sb(f'c_dS{j}', [64, 4, 4, 64], F32)
            d_['dec'] = sb(f'c_dec{j}', [64, 4, 4], F32)
            d_['vb'] = sb(f'c_vb{j}', [128, 256], BF16)
            d_['sgl'] = sb(f'c_sgl{j}', [128, 256], F32)
            d_['Sbf4'] = sb(f'c_Sbf4{j}', [64, 4, 4, 64], BF16)
            slots.append(d_)
        oTsr = Ring(es, nc, 'c_oTs', [64, 4, 128], F32, 2)
        oFr = Ring(es, nc, 'c_oF', [64, 4, 128], F32, 2)
        sqr = Ring(es, nc, 'c_sq', [128, 4, 64], F32, 2)
        ofr = Ring(es, nc, 'c_of', [128, 4, 64], F32, 2)
        onr = Ring(es, nc, 'c_on', [128, 4, 64], F32, 2)
        ocr = Ring(es, nc, 'c_oc', [128, 256], BF16, 2)
        ss4r = Ring(es, nc, 'c_ss4', [128, 4], F32, 2)
        ln4r = Ring(es, nc, 'c_ln4', [128, 4], F32, 2)
        rs4r = Ring(es, nc, 'c_rs4', [128, 4], F32, 2)

        def prep(i, d, sl):
            mi = 0 if d == 0 else 1
            mx = 2 if d == 0 else 3
            hT = hTr.next()
            S.dma('sp', hT, k.hT_d[i])
            proj(k, p1, hT, wC, 0, 512)
            proj(k, p2[:, 0:256], hT, wC, 512 + 256 * d, 768 + 256 * d)
            if d == 1:
                proj(k, p2[:, 256:512], hT, wC, 1024, 1280)
            qh = qhr.next()
            S.act(qh, p1[:, 0:256], AF.Silu)
            S.copy('dve', sl['vb'], p1[:, 256:512])
            sg = sgr.next()
            S.act(sg, p2[:, 0:256], AF.Sigmoid)
            if d == 1:
                S.act(sl['sgl'], p2[:, 256:512], AF.Silu)
            if l > 0:
                f = fr.next()
                S.tt('dve', f, sg, OML[:, d, :], ALU.mult)
                S.tt('dve', f, f, LBt[:, d, :], ALU.add)
            else:
                f = sg
            kk_ = kkr.next()
            S.ts('dve', kk_, f, -1.0, ALU.mult, 1.0, ALU.add)
            lf = lfr.next()
            S.act(lf, f, AF.Ln)
            S.mm(pb[:, 0:256], gm[:, mi, :], lf)
            S.mm(pb[:, 256:512], gm[:, mx, :], lf)
            e1, e2, e3 = e1r.next(), e2r.next(), e3r.next()
            S.act(e1, pb[:, 0:256], AF.Exp)
            S.act(e2, pb[:, 0:256], AF.Exp, scale=-1.0)
            S.act(e3, pb[:, 256:512], AF.Exp)
            qd, kd, krf, kr4 = qdr.next(), kdr.next(), krfr.next(), kr4r.next()
            S.tt('dve', qd, qh, e1, ALU.mult)
            S.tt('dve', kd, kk_, e2, ALU.mult)
            S.tt('dve', krf, kk_, e3, ALU.mult)
            S.tt('dve', kr4, krf.unsqueeze(1).to_broadcast([128, 4, 256]),
                 ci.unsqueeze(2).to_broadcast([128, 4, 256]), ALU.mult)
            for h in range(4):
                S.mm(pm[0:64, 256 + h * 4:260 + h * 4], lf[:, h * 64:(h + 1) * 64], ci)
            S.act(sl['dec'], pm[0:64, 256:272].rearrange("p (h c) -> p h c", h=4), AF.Exp)
            pTv = pT[0:64, :].rearrange("p (w t) -> p w t", w=8)
            for h in range(4):
                S.tr(pTv[:, h, :], qd[:, h * 64:(h + 1) * 64], k.ident_b)
                S.tr(pTv[:, 4 + h, :], kd[:, h * 64:(h + 1) * 64], k.ident_b)
            S.copy('dve', sl['qkT'], pTv)
            for h in range(4):
                S.mm(pSc[:, h * 128:(h + 1) * 128], sl['qkT'][:, 4 + h, :], sl['qkT'][:, h, :])
            S.tt('dve', sl['A'], pSc.rearrange("p (h t) -> p h t", h=4),
                 gm[:, mi, :].unsqueeze(1).to_broadcast([128, 4, 128]), ALU.mult)
            for half in range(2):
                for cc in range(2):
                    c = 2 * half + cc
                    for h in range(4):
                        S.mm(pdS[0:64, (cc * 4 + h) * 64:(cc * 4 + h + 1) * 64], kr4[:, c, h * 64:(h + 1) * 64],
                             sl['vb'][:, h * 64:(h + 1) * 64])
                S.copy('act', sl['dS'][:, 2 * half:2 * half + 2, :, :],
                       pdS[0:64, :].rearrange("p (c h v) -> p c h v", c=2, h=4))

        def chain(i, d, sl):
            for h in range(4):
                S.op('pe', lambda e, h=h: e.matmul(poT[0:64, h * 128:(h + 1) * 128], sl['vb'][:, h * 64:(h + 1) * 64],
                                                   sl['A'][:, h, :], start=(h == 0), stop=False, skip_group_check=True),
                     [sl['vb'][:, h * 64:(h + 1) * 64], sl['A'][:, h, :]], [poT])
            corder = [0, 1, 2, 3] if d == 0 else [3, 2, 1, 0]
            for n, c in enumerate(corder):
                S.copy('dve', sl['Sbf4'][:, c, :, :], S_st)
                S.tt('dve', tmpS, S_st, sl['dec'][:, :, c].unsqueeze(2).to_broadcast([64, 4, 64]), ALU.mult)
                S.tt('dve', S_st, tmpS, sl['dS'][:, c, :, :], ALU.add)
            for n, c in enumerate(corder):
                for h in range(4):
                    S.op('pe', lambda e, h=h, c=c, n=n: e.matmul(
                        poT[0:64, h * 128 + c * 32:h * 128 + (c + 1) * 32], sl['Sbf4'][:, c, h, :],
                        sl['qkT'][:, h, c * 32:(c + 1) * 32],
                        start=False, stop=(n == 3 and h == 3), skip_group_check=True),
                        [sl['Sbf4'][:, c, h, :], sl['qkT'][:, h, c * 32:(c + 1) * 32]], [poT])
            oTs = oTsr.next()
            S.copy('act', oTs, poT[0:64, :].rearrange("p (h t) -> p h t", h=4))
            if d == 0:
                S.dma('sp', k.oF_d[i], oTs)
                return
            if i < k.t0:
                return
            oF = oFr.next()
            S.dma('sp', oF, k.oF_d[i])
            S.tt('dve', oTs, oTs, oF, ALU.add)
            pmv = pm[:, 0:256].rearrange("p (h v) -> p h v", h=4)
            for h in range(4):
                S.tr(pmv[:, h, :], oTs[:, h, :], k.ident_f[0:64, 0:64])
            of = ofr.next()
            S.copy('act', of, pmv)
            pmv = of
            sq = sqr.next()
            S.tt('dve', sq, pmv, pmv, ALU.mult)
            ss4 = ss4r.next()
            S.op('dve', lambda e: e.tensor_reduce(ss4, sq, AX.X, ALU.add), [sq], [ss4])
            rs4 = rs4r.next()
            rsqrt_act(k, rs4, ss4, ln4r.next(), 1.0 / 64.0, EPS)
            on = onr.next()
            S.tt('dve', on, pmv, rs4.unsqueeze(2).to_broadcast([128, 4, 64]), ALU.mult)
            S.tt('dve', on, on, NW.unsqueeze(1).to_broadcast([128, 4, 64]), ALU.mult)
            oc = ocr.next()
            S.tt('dve', oc, on.rearrange("p h v -> p (h v)"), sl['sgl'], ALU.mult)
            S.dma('sp', k.cat_d[i * 128:(i + 1) * 128, 512:768], oc)

        for d in range(2):
            order = list(range(NT)) if d == 0 else [1, 0] + list(range(NT - 1, 1, -1))
            S.memset('dve', S_st, 0.0)
            prep(order[0], d, slots[0])
            for n, i in enumerate(order):
                if n + 1 < len(order):
                    prep(order[n + 1], d, slots[(n + 1) % 2])
                chain(i, d, slots[n % 2])
    S.barrier()


def load_w_generic(k, dst, src, stage):
    n = src.shape[1]
    j = 0
    for a in range(0, n, 256):
        b = min(n, a + 256)
        st = stage.next()
        k.S.dma('sp', st[:, :, 0:b - a], src[:, a:b].rearrange("(kk p) n -> p kk n", p=128))
        k.S.copy('act' if j % 2 else 'dve', dst[:, :, a:b], st[:, :, 0:b - a])
        j += 1


def residual_ln(k, y_halves, gate_t, x_tile, lw, lb, out_t, z, zn, st):
    S = k.S
    for cb in range(2):
        S.tt('dve', z[:, cb * 512:(cb + 1) * 512], y_halves[cb], gate_t[:, cb * 512:(cb + 1) * 512], ALU.mult)
    S.stt('dve', z, x_tile, ALPHA, z, ALU.mult, ALU.add)
    ln_stats(k, z, D, st)
    S.act(zn, z, AF.Identity, bias=st['nmr'], scale=st['rstd'])
    S.tt('dve', zn, zn, lw, ALU.mult)
    S.tt('dve', out_t, zn, lb, ALU.add)


def phase_O(k):
    nc, S, l = k.nc, k.S, k.l
    with ExitStack() as es:
        sb = lambda n, s, d: es.enter_context(nc.sbuf_tensor(UN(n), list(s), d)).ap()
        stg = wstage(es, nc, 'o_stg')
        wO = sb('o_w', [128, 8, 1024], BF16)
        load_w_generic(k, wO, k.w_out[l], stg)
        G1 = sb('o_G1', [128, 2, D], F32)
        for r in range(2):
            S.dma('sp', G1[:, r, :], k.gate_d[l, r:r + 1, 0, :].to_broadcast([128, D]))
        LW = sb('o_LW', [128, D], F32)
        LB = sb('o_LB', [128, D], F32)
        S.dma('sp', LW, k.ln_mix_w[l:l + 1, :].to_broadcast([128, D]))
        S.dma('sp', LB, k.ln_mix_b[l:l + 1, :].to_broadcast([128, D]))
        rw = sb('o_rw', [128, 8, NEXP], F32)
        S.dma('sp', rw, k.router_w[l].rearrange("(kk p) e -> p kk e", p=128))
        RB = sb('o_RB', [128, NEXP], F32)
        S.dma('sp', RB, k.router_b[l:l + 1, :].to_broadcast([128, NEXP]))
        ctr = Ring(es, nc, 'o_ct', [128, D], BF16, 2)
        cTr = Ring(es, nc, 'o_cT', [128, 8, 128], BF16, 2)
        xtr = Ring(es, nc, 'o_xt', [128, D], F32, 2)
        zr = Ring(es, nc, 'o_z', [128, D], F32, 2)
        znr = Ring(es, nc, 'o_zn', [128, D], F32, 2)
        x1r = Ring(es, nc, 'o_x1', [128, D], F32, 2)
        xn2r = Ring(es, nc, 'o_xn2', [128, D], F32, 2)
        h2fr = Ring(es, nc, 'o_h2f', [128, 8, 128], F32, 2)
        h2br = Ring(es, nc, 'o_h2b', [128, 8, 128], BF16, 2)
        pT = Ring(es, nc, 'o_pT', [128, 1024], BF16, 1, psum=True)
        pY = Ring(es, nc, 'o_pY', [128, 512], F32, 4, psum=True)
        pFt = Ring(es, nc, 'o_pF', [128, 512], F32, 2, psum=True)
        pL = Ring(es, nc, 'o_pL', [128, 512], F32, 1, psum=True)
        sts = stat_tiles(es, nc, 'o', 4)
        lgr = Ring(es, nc, 'o_lg', [128, NEXP], F32, 2)
        t8r = Ring(es, nc, 'o_t8', [128, 8], F32, 2)
        mkr = Ring(es, nc, 'o_mk', [128, NEXP], F32, 2)
        exr = Ring(es, nc, 'o_ex', [128, NEXP], F32, 2)
        smr = Ring(es, nc, 'o_sm', [128, 2], F32, 2)
        def stage_a(i):
            ct = ctr.next()
            S.dma('sp', ct, k.cat_d[i * 128:(i + 1) * 128, :])
            pt = pT.next()
            ptv = pt.rearrange("p (kk t) -> p kk t", kk=8)
            for kk in range(8):
                S.tr(ptv[:, kk, :], ct[:, kk * 128:(kk + 1) * 128], k.ident_b)
            cT = cTr.next()
            S.copy('act', cT[:, 0:4, :], ptv[:, 0:4, :])
            S.copy('dve', cT[:, 4:8, :], ptv[:, 4:8, :])
            py = [pY.next(), pY.next()]
            for cb in range(2):
                for kk in range(8):
                    S.mm(py[cb], cT[:, kk, :], wO[:, kk, cb * 512:(cb + 1) * 512], start=(kk == 0), stop=(kk == 7))
            xt = xtr.next()
            S.dma('sp', xt, x_src(k, i))
            return py, xt

        def stage_b(i, py, xt):
            r = mcol(k, i)
            x1 = x1r.next()
            residual_ln(k, py, G1[:, r, :], xt, LW, LB, x1, zr.next(), znr.next(), sts[(2 * i) % 4])
            S.dma('sp', k.xs_d[i * 128:(i + 1) * 128, :], x1)
            st2 = sts[(2 * i + 1) % 4]
            ln_stats(k, x1, D, st2)
            xn2 = xn2r.next()
            S.act(xn2, x1, AF.Identity, bias=st2['nmr'], scale=st2['rstd'])
            pf = [pFt.next(), pFt.next()]
            for kk in range(8):
                S.tr(pf[kk // 4][:, (kk % 4) * 128:(kk % 4 + 1) * 128], xn2[:, kk * 128:(kk + 1) * 128], k.ident_f)
            h2f = h2fr.next()
            for kk in range(8):
                sc = k.modT[:, 32 + kk, r:r + 1]
                sh = k.modT[:, 24 + kk, r:r + 1]
                src = pf[kk // 4][:, (kk % 4) * 128:(kk % 4 + 1) * 128]
                if kk % 2 == 0:
                    S.act(h2f[:, kk, :], src, AF.Identity, bias=sh, scale=sc)
                else:
                    S.ts('dve', h2f[:, kk, :], src, sc, ALU.mult, sh, ALU.add)
            pl = pL.next()
            for kk in range(8):
                S.mm(pl[:, 0:NEXP], h2f[:, kk, :], rw[:, kk, :], start=(kk == 0), stop=(kk == 7))
            h2b = h2br.next()
            S.copy('act', h2b, h2f)
            S.dma('sp', k.hT_d[i], h2b)
            lg = lgr.next()
            S.tt('dve', lg, pl[:, 0:NEXP], RB, ALU.add)
            t8 = t8r.next()
            S.op('dve', lambda e, t8=t8, lg=lg: e.max(t8, lg), [lg], [t8])
            mk = mkr.next()
            S.ts('dve', mk, lg, t8[:, 3:4], ALU.is_ge)
            sm = smr.next()
            S.ts('dve', sm[:, 0:1], t8[:, 0:1], -1.0, ALU.mult)
            ex = exr.next()
            S.act(ex, lg, AF.Exp, bias=sm[:, 0:1])
            S.tt('dve', ex, ex, mk, ALU.mult)
            S.op('dve', lambda e, sm=sm, ex=ex: e.tensor_reduce(sm[:, 1:2], ex, AX.X, ALU.add), [ex], [sm[:, 1:2]])
            S.op('dve', lambda e, sm=sm: e.reciprocal(sm[:, 1:2], sm[:, 1:2]), [sm[:, 1:2]], [sm[:, 1:2]])
            S.ts('dve', k.gates[:, i, :], ex, sm[:, 1:2], ALU.mult)
        tl = list(range(k.t0, NT))
        nxt = stage_a(tl[0])
        for n, i in enumerate(tl):
            cur = nxt
            if n + 1 < len(tl):
                nxt = stage_a(tl[n + 1])
            stage_b(i, cur[0], cur[1])
    S.barrier()


def phase_M(k):
    nc, S, l = k.nc, k.S, k.l
    li = k.cfg.get('layers', [0, 1]).index(l)
    while k.conv_list and k.conv_list[0][0] <= li:
        k.conv_step(None)
    nexp = k.nexp
    tiles = list(range(k.t0, NT))
    GS = 12
    groups = [tiles[a:a + GS] for a in range(0, len(tiles), GS)]
    last = (l == DEPTH - 1)
    with ExitStack() as es:
        sb = lambda n, s, d: es.enter_context(nc.sbuf_tensor(UN(n), list(s), d)).ap()
        pg = Ring(es, nc, 'm_pg', [128, 512], F32, 2, psum=True)
        pl = Ring(es, nc, 'm_pl', [128, 512], F32, 2, psum=True)
        pY = Ring(es, nc, 'm_pY', [128, 512], F32, 2, psum=True)
        pX = Ring(es, nc, 'm_pX', [128, 512], F32, 1, psum=True)
        B1T = sb('m_B1T', [128, 16, nexp], F32)
        with ExitStack() as esb:
            b1raw = esb.enter_context(nc.sbuf_tensor(UN('m_b1raw'), [nexp, 2 * D], F32)).ap()
            S.dma('sp', b1raw, k.b1[li])
            px = pX.next()
            pxv = px[:, 0:16 * nexp].rearrange("p (j e) -> p j e", j=16)
            for fb in range(8):
                for two in range(2):
                    S.tr(pxv[:, fb * 2 + two, :], b1raw[0:nexp, fb * 256 + two:fb * 256 + 256:2], k.ident_f[0:nexp, 0:nexp])
            S.copy('dve', B1T, pxv)
        S.barrier()
        b2f = sb('m_b2f', [nexp, D], F32)
        S.dma('sp', b2f, k.b2[li])
        G2 = sb('m_G2', [128, 2, D], F32)
        for r in range(2):
            S.dma('sp', G2[:, r, :], k.gate_d[l, r:r + 1, 1, :].to_broadcast([128, D]))
        LW = sb('m_LW', [128, D], F32)
        LB = sb('m_LB', [128, D], F32)
        S.dma('sp', LW, k.ln_ffn_w[l:l + 1, :].to_broadcast([128, D]))
        S.dma('sp', LB, k.ln_ffn_b[l:l + 1, :].to_broadcast([128, D]))
        acc = sb('m_acc', [128, GS, D], F32)
        h2g = sb('m_h2g', [128, 8, GS * 128], BF16)
        aT = sb('m_aT', [128, 8, GS * 128], BF16)
        w1r = Ring(es, nc, 'm_w1', [128, 8, 256], BF16, 3)
        w2r = Ring(es, nc, 'm_w2', [128, 8, D], BF16, 2)
        gr = Ring(es, nc, 'm_g', [128, 512], F32, 2)
        sr = Ring(es, nc, 'm_s', [128, 512], F32, 2)
        lr = Ring(es, nc, 'm_l', [128, 512], F32, 2)
        gTr = Ring(es, nc, 'm_gT', [nexp, 128], F32, 2)
        xtr = Ring(es, nc, 'm_xt', [128, D], F32, 1)
        zr = Ring(es, nc, 'm_z', [128, D], F32, 1)
        znr = Ring(es, nc, 'm_zn', [128, D], F32, 1)
        xor_ = Ring(es, nc, 'm_xo', [128, D], F32, 2)
        sts = stat_tiles(es, nc, 'm', 2)
        for grp in groups:
            n = len(grp)
            ntok = n * 128
            for j, i in enumerate(grp):
                S.dma('sp', h2g[:, :, j * 128:(j + 1) * 128], k.hT_d[i])
            for e in range(nexp):
                w2e = w2r.next()
                S.dma('sp', w2e, k.w2b_d[li, e].rearrange("(fc p) d -> p fc d", p=128))
                for fb in range(8):
                    w1s = w1r.next()
                    S.dma('sp', w1s, k.w1b_d[li, e, :, fb * 256:(fb + 1) * 256].rearrange("(kk p) c -> p kk c", p=128))
                    b1g = B1T[:, fb * 2, e:e + 1]
                    b1l = B1T[:, fb * 2 + 1, e:e + 1]
                    for t0 in range(0, ntok, 512):
                        nb = min(512, ntok - t0)
                        a, b = pg.next(), pl.next()
                        for kk in range(8):
                            S.mm(a[:, 0:nb], w1s[:, kk, 0:256:2], h2g[:, kk, t0:t0 + nb], start=(kk == 0), stop=(kk == 7))
                        for kk in range(8):
                            S.mm(b[:, 0:nb], w1s[:, kk, 1:256:2], h2g[:, kk, t0:t0 + nb], start=(kk == 0), stop=(kk == 7))
                        g = gr.next()
                        S.ts('dve', g[:, 0:nb], a[:, 0:nb], b1g, ALU.add, SW_LIM, ALU.min)
                        s_ = sr.next()
                        S.act(s_[:, 0:nb], g[:, 0:nb], AF.Sigmoid, scale=SW_ALPHA)
                        lt = lr.next()
                        S.act(lt[:, 0:nb], b[:, 0:nb], AF.Identity, bias=b1l)
                        S.ts('dve', lt[:, 0:nb], lt[:, 0:nb], SW_LIM, ALU.min, -SW_LIM, ALU.max)
                        S.tt('dve', g[:, 0:nb], g[:, 0:nb], s_[:, 0:nb], ALU.mult)
                        S.stt('dve', aT[:, fb, t0:t0 + nb], lt[:, 0:nb], 1.0, g[:, 0:nb], ALU.add, ALU.mult)
                for j, i in enumerate(grp):
                    gate = k.gates[:, i, e:e + 1]
                    for cb in range(2):
                        py = pY.next()
                        for fb in range(8):
                            S.mm(py, aT[:, fb, j * 128:(j + 1) * 128], w2e[:, fb, cb * 512:(cb + 1) * 512],
                                 start=(fb == 0), stop=(fb == 7))
                        dst = acc[:, j, cb * 512:(cb + 1) * 512]
                        if e == 0:
                            S.ts('dve', dst, py, gate, ALU.mult)
                        else:
                            S.stt('dve', dst, py, gate, dst, ALU.mult, ALU.add)
            for j, i in enumerate(grp):
                r = mcol(k, i)
                px = pX.next()
                S.tr(px[0:nexp, 0:128], k.gates[:, i, 0:nexp], k.ident_f)
                gT = gTr.next()
                S.copy('act', gT, px[0:nexp, 0:128])
                pyb = [pY.next(), pY.next()]
                for cb in range(2):
                    S.mm(pyb[cb], gT, b2f[0:nexp, cb * 512:(cb + 1) * 512])
                    S.tt('dve', acc[:, j, cb * 512:(cb + 1) * 512], acc[:, j, cb * 512:(cb + 1) * 512], pyb[cb], ALU.add)
                xt = xtr.next()
                S.dma('sp', xt, k.xs_d[i * 128:(i + 1) * 128, :])
                xo = xor_.next()
                residual_ln(k, [acc[:, j, 0:512], acc[:, j, 512:1024]], G2[:, r, :], xt, LW, LB, xo, zr.next(), znr.next(),
                            sts[j % 2])
                if last:
                    S.dma('sp', k.y[(i - 2) * 128:(i - 1) * 128, :], xo)
                else:
                    S.dma('sp', k.xs_d[i * 128:(i + 1) * 128, :], xo)
    S.barrier()


_W_KEYS = ['ada_w', 'ada_b', 'w_in', 'w_out', 'sgu_ln_w', 'sgu_ln_b', 'sgu_w', 'sgu_b', 'diff_lambda', 'diff_subln_w',
           'hgrn_lower_bounds', 'hgrn_norm_w', 'mla_q_norm_w', 'mla_w_uq', 'mla_kv_norm_w', 'mla_w_ukv',
           'ln_mix_w', 'ln_mix_b', 'ln_ffn_w', 'ln_ffn_b', 'router_w', 'router_b',
           'expert_w1', 'expert_b1', 'expert_w2', 'expert_b2']


def make_in_maps(inputs, cores, layers=(0, 1)):
    f = lambda a: np.ascontiguousarray(np.asarray(a), dtype=np.float32)
    consts = host_consts()
    shared = {kk: f(inputs[kk]) for kk in _W_KEYS}
    if tuple(layers) != (0, 1):
        for kk in ('expert_w1', 'expert_b1', 'expert_w2', 'expert_b2'):
            shared[kk] = np.ascontiguousarray(shared[kk][list(layers)])
    x = np.asarray(inputs['x'])
    ctx = np.asarray(inputs['ctx'])
    c = np.asarray(inputs['c'])
    cc = np.asarray(inputs['c_ctx'])
    maps = []
    for i in cores:
        m = dict(shared)
        m.update(consts)
        m['x'] = f(x[i])
        m['ctx'] = f(ctx[i])
        m['cvec'] = f(np.stack([c[i], cc], 0))
        maps.append(m)
    return maps


def kernel(**inputs):
    nc, k = build_program({})
    maps = make_in_maps(inputs, range(8))
    res = run_bass_kernel_spmd(nc, maps, core_ids=list(range(8)))
    return np.stack([np.asarray(r['y'], dtype=np.float32) for r in res.results], 0)
```
